# Optimizing a Trainium2 kernel written in Bass

```python
import jax, jax.numpy as jnp
from jax import lax
import numpy as np


D_MODEL = 2048
BATCH = 2
SEQ = 4096
DEPTH = 1
DEC_BATCH = 4
DEC_SEQ = 4096
PAST_LEN = 128

HG_DK = 128
HG_DV = 128
HG_HEADS = D_MODEL // HG_DK
HG_FDIM = HG_HEADS * HG_DK
HG_WIDTH = HG_HEADS * HG_DV
CHUNK = 64
CONV_WIDTH = D_MODEL // 2
CONV_K = 31
N_EXPERTS = 32
TOP_K = 4
D_FF = D_MODEL
SWIGLU_ALPHA = 1.702
SWIGLU_LIMIT = 7.0
PLE_DIM = 256
DN_ALPHA = (2.0 * DEPTH) ** 0.25
DN_BETA = (8.0 * DEPTH) ** -0.25
NORM_EPS = 1e-5
IN_SIZES = (HG_FDIM, HG_FDIM, HG_FDIM, HG_WIDTH, HG_WIDTH, 2 * CONV_WIDTH, D_MODEL, D_MODEL)
IN_DIM = sum(IN_SIZES)
SPLIT_POINTS = tuple(int(v) for v in np.cumsum(IN_SIZES)[:-1])

kernel_name = 'hgrn2_conformer_moe_encoder'


def _layer_norm(x, g, b):
    xf = x.astype(jnp.float32)
    mu = jnp.mean(xf, axis=-1, keepdims=True)
    xc = xf - mu
    var = jnp.mean(xc * xc, axis=-1, keepdims=True)
    return (xc * lax.rsqrt(var + NORM_EPS) * g + b).astype(x.dtype)


def _rms_norm(x, g):
    xf = x.astype(jnp.float32)
    return (xf * lax.rsqrt(jnp.mean(xf * xf, axis=-1, keepdims=True) + NORM_EPS) * g).astype(x.dtype)


def _chunk_scan(q, k, v, logf):
    B, S = q.shape[0], q.shape[1]
    nc = S // CHUNK

    def to_chunks(t):
        return t.reshape(B, nc, CHUNK, HG_HEADS, t.shape[-1]).transpose(1, 0, 3, 2, 4)

    qc, kc, vc, gc = (to_chunks(t) for t in (q, k, v, logf))
    mask = jnp.tril(jnp.ones((CHUNK, CHUNK), dtype=bool))[:, :, None]

    def step(state, inp):
        qi, ki, vi, gi = inp
        b = jnp.cumsum(gi, axis=2)
        b_last = b[:, :, -1:, :]
        diff = b[:, :, :, None, :] - b[:, :, None, :, :]
        decay = jnp.exp(jnp.where(mask, diff, -jnp.inf))
        scores = jnp.einsum('bhtk,bhtsk,bhsk->bhts', qi, decay, ki)
        o_intra = jnp.einsum('bhts,bhsv->bhtv', scores, vi)
        o_inter = jnp.einsum('bhtk,bhkv->bhtv', qi * jnp.exp(b), state)
        k_dec = ki * jnp.exp(b_last - b)
        new_state = jnp.exp(b_last[:, :, 0, :])[..., None] * state + jnp.einsum('bhsk,bhsv->bhkv', k_dec, vi)
        return new_state, o_intra + o_inter

    s0 = jnp.zeros((B, HG_HEADS, HG_DK, HG_DV), jnp.float32)
    _, o = lax.scan(step, s0, (qc, kc, vc, gc))
    return o.transpose(1, 0, 3, 2, 4).reshape(B, S, HG_HEADS, HG_DV)


def _hgrn2_branch(q, f_fwd, f_bwd, v, og, lb, norm_g, w_o):
    B, S, _ = q.shape

    def heads(t):
        return t.astype(jnp.float32).reshape(B, S, HG_HEADS, -1)

    qh = heads(jax.nn.silu(q))
    vh = heads(v)

    def one_direction(f_logits, lb_d, reverse):
        gate = lb_d + (1.0 - lb_d) * jax.nn.sigmoid(f_logits.astype(jnp.float32))
        kh = heads(1.0 - gate)
        gh = heads(jnp.log(gate))
        if reverse:
            o = _chunk_scan(*(jnp.flip(t, axis=1) for t in (qh, kh, vh, gh)))
            return jnp.flip(o, axis=1)
        return _chunk_scan(qh, kh, vh, gh)

    o = one_direction(f_fwd, lb[0], False) + one_direction(f_bwd, lb[1], True)
    o = _rms_norm(o.reshape(B, S, HG_WIDTH), norm_g) * jax.nn.silu(og.astype(jnp.float32))
    return o.astype(q.dtype) @ w_o


def _conformer_conv(u, dw_w, dw_b, ln_g, ln_b, w_pw):
    a, g = jnp.split(u, 2, axis=-1)
    h = a * jax.nn.sigmoid(g)
    h = lax.conv_general_dilated(
        h, dw_w[:, None, :], window_strides=(1,),
        padding=[(CONV_K // 2, CONV_K // 2)],
        dimension_numbers=('NWC', 'WIO', 'NWC'),
        feature_group_count=CONV_WIDTH) + dw_b
    h = jax.nn.silu(_layer_norm(h, ln_g, ln_b))
    return h @ w_pw


def _moe(x, w_r, b_r, w1, b1, w2, b2):
    shape = x.shape
    t = x.reshape(-1, shape[-1])
    logits = (t @ w_r + b_r).astype(jnp.float32)
    top_v, top_i = lax.top_k(logits, TOP_K)
    top_w = jax.nn.softmax(top_v, axis=-1)
    gates = jnp.einsum('tk,tke->te', top_w, jax.nn.one_hot(top_i, N_EXPERTS, dtype=jnp.float32))
    out = jnp.zeros(t.shape, jnp.float32)
    for e in range(N_EXPERTS):
        h = t @ w1[e] + b1[e]
        hg, hl = jnp.split(h, 2, axis=-1)
        hg = jnp.minimum(hg, SWIGLU_LIMIT)
        hl = jnp.clip(hl, -SWIGLU_LIMIT, SWIGLU_LIMIT)
        act = hg * jax.nn.sigmoid(SWIGLU_ALPHA * hg) * (hl + 1.0)
        out = out + gates[:, e:e + 1] * (act @ w2[e] + b2[e])
    return out.astype(x.dtype).reshape(shape)


def _encoder_layer(x, p_l, lb, w_in, hg_norm_g, w_hg_out, conv_w, conv_b, conv_ln_g, conv_ln_b,
                   w_conv_out, w_out, ln1_g, ln1_b, w_router, b_router, w_exp1, b_exp1,
                   w_exp2, b_exp2, w_ple_gate, w_ple_proj, ln2_g, ln2_b):
    proj = x @ w_in
    q, f_f, f_b, iv, og, glu, ga, gc = jnp.split(proj, SPLIT_POINTS, axis=-1)
    a = _hgrn2_branch(q, f_f, f_b, iv, og, lb, hg_norm_g, w_hg_out)
    c = _conformer_conv(glu, conv_w, conv_b, conv_ln_g, conv_ln_b, w_conv_out)
    mixed = (jax.nn.sigmoid(ga) * a + jax.nn.sigmoid(gc) * c) @ w_out
    x = _layer_norm(DN_ALPHA * x + mixed, ln1_g, ln1_b)
    ple = jax.nn.sigmoid(x @ w_ple_gate) * (p_l @ w_ple_proj)
    x = _layer_norm(DN_ALPHA * x + _moe(x, w_router, b_router, w_exp1, b_exp1, w_exp2, b_exp2) + ple,
                    ln2_g, ln2_b)
    return x


def _trunk(x, p, ln_emb_g, ln_emb_b, w_in, lower_bounds, hg_norm_g, w_hg_out, conv_w, conv_b,
           conv_ln_g, conv_ln_b, w_conv_out, w_out, ln1_g, ln1_b, w_router, b_router, w_exp1,
           b_exp1, w_exp2, b_exp2, w_ple_gate, w_ple_proj, ln2_g, ln2_b):
    lbs = jnp.cumsum(jax.nn.softmax(lower_bounds.astype(jnp.float32), axis=1), axis=1)
    h = _layer_norm(x, ln_emb_g, ln_emb_b)
    for l in range(DEPTH):
        h = _encoder_layer(h, p[l], lbs[:, l], w_in[l], hg_norm_g[l], w_hg_out[l], conv_w[l], conv_b[l],
                           conv_ln_g[l], conv_ln_b[l], w_conv_out[l], w_out[l], ln1_g[l], ln1_b[l],
                           w_router[l], b_router[l], w_exp1[l], b_exp1[l], w_exp2[l], b_exp2[l],
                           w_ple_gate[l], w_ple_proj[l], ln2_g[l], ln2_b[l])
    return h


def setup_inputs(seed: int = 0) -> dict:
    key = jax.random.key(seed)
    ks = jax.random.split(key, 32)

    def n(k, s):
        return jax.random.normal(k, s, jnp.float32)

    return {
        'x_prompt': n(ks[0], (BATCH, SEQ, D_MODEL)),
        'x_sample': n(ks[1], (DEC_BATCH, DEC_SEQ, D_MODEL)),
        'p_prompt': n(ks[2], (DEPTH, BATCH, SEQ, PLE_DIM)),
        'p_sample': n(ks[3], (DEPTH, DEC_BATCH, DEC_SEQ, PLE_DIM)),
        'ln_emb_g': 1.0 + 0.02 * n(ks[4], (D_MODEL,)),
        'ln_emb_b': 0.02 * n(ks[5], (D_MODEL,)),
        'w_in': n(ks[6], (DEPTH, D_MODEL, IN_DIM)) * D_MODEL ** -0.5,
        'lower_bounds': 0.1 * n(ks[7], (2, DEPTH + 1, HG_FDIM)),
        'hg_norm_g': 1.0 + 0.02 * n(ks[8], (DEPTH, HG_WIDTH)),
        'w_hg_out': n(ks[9], (DEPTH, HG_WIDTH, D_MODEL)) * (DN_BETA * HG_WIDTH ** -0.5),
        'conv_w': n(ks[10], (DEPTH, CONV_K, CONV_WIDTH)) * CONV_K ** -0.5,
        'conv_b': 0.02 * n(ks[11], (DEPTH, CONV_WIDTH)),
        'conv_ln_g': 1.0 + 0.02 * n(ks[12], (DEPTH, CONV_WIDTH)),
        'conv_ln_b': 0.02 * n(ks[13], (DEPTH, CONV_WIDTH)),
        'w_conv_out': n(ks[14], (DEPTH, CONV_WIDTH, D_MODEL)) * (DN_BETA * CONV_WIDTH ** -0.5),
        'w_out': n(ks[15], (DEPTH, D_MODEL, D_MODEL)) * (DN_BETA * D_MODEL ** -0.5),
        'ln1_g': 1.0 + 0.02 * n(ks[16], (DEPTH, D_MODEL)),
        'ln1_b': 0.02 * n(ks[17], (DEPTH, D_MODEL)),
        'w_router': n(ks[18], (DEPTH, D_MODEL, N_EXPERTS)) * D_MODEL ** -0.5,
        'b_router': 0.01 * n(ks[19], (DEPTH, N_EXPERTS)),
        'w_exp1': n(ks[20], (DEPTH, N_EXPERTS, D_MODEL, 2 * D_FF)) * D_MODEL ** -0.5,
        'b_exp1': 0.02 * n(ks[21], (DEPTH, N_EXPERTS, 2 * D_FF)),
        'w_exp2': n(ks[22], (DEPTH, N_EXPERTS, D_FF, D_MODEL)) * (DN_BETA * D_FF ** -0.5),
        'b_exp2': 0.02 * n(ks[23], (DEPTH, N_EXPERTS, D_MODEL)),
        'w_ple_gate': n(ks[24], (DEPTH, D_MODEL, D_MODEL)) * D_MODEL ** -0.5,
        'w_ple_proj': n(ks[25], (DEPTH, PLE_DIM, D_MODEL)) * (DN_BETA * PLE_DIM ** -0.5),
        'ln2_g': 1.0 + 0.02 * n(ks[26], (DEPTH, D_MODEL)),
        'ln2_b': 0.02 * n(ks[27], (DEPTH, D_MODEL)),
    }


def reference(x_prompt, x_sample, p_prompt, p_sample, ln_emb_g, ln_emb_b, w_in, lower_bounds,
              hg_norm_g, w_hg_out, conv_w, conv_b, conv_ln_g, conv_ln_b, w_conv_out, w_out,
              ln1_g, ln1_b, w_router, b_router, w_exp1, b_exp1, w_exp2, b_exp2,
              w_ple_gate, w_ple_proj, ln2_g, ln2_b):
    shared = (ln_emb_g, ln_emb_b, w_in, lower_bounds, hg_norm_g, w_hg_out, conv_w, conv_b,
              conv_ln_g, conv_ln_b, w_conv_out, w_out, ln1_g, ln1_b, w_router, b_router,
              w_exp1, b_exp1, w_exp2, b_exp2, w_ple_gate, w_ple_proj, ln2_g, ln2_b)
    y_prompt = _trunk(x_prompt, p_prompt, *shared)
    y_sample = _trunk(x_sample, p_sample, *shared)
    return (y_prompt, y_sample)
```

```python
import numpy as np
import concourse.bass as bass
import concourse.mybir as mybir
from concourse.bass_utils import run_bass_kernel_spmd

F32 = mybir.dt.float32
BF16 = mybir.dt.bfloat16
AF = mybir.ActivationFunctionType
ALU = mybir.AluOpType

D = 2048
NH = 16
EPS = 1e-5
ALPHA = 2.0 ** 0.25
TOPK = 4
ENG = ("sp", "act", "dve", "pool", "pe")


class Sched:
    def __init__(self, nc):
        self.nc = nc
        self.ops = {e: [] for e in ENG}
        self.cnt = {e: 0 for e in ENG}
        self.epoch = {e: 0 for e in ENG}
        self.sem = {e: nc.alloc_semaphore(name=f"sem_{e}_0") for e in ENG}
        self.known = {e: {} for e in ENG}
        self.buf = {}
        self.dsem = {}
        self.last = {}

    def _deps(self, eng, r, w, use_known=True):
        need = []
        for k in r:
            b = self.buf.get(k)
            if b and b[0]:
                need.append((b[0], True))
        for k in w:
            b = self.buf.get(k)
            if b:
                if b[0]:
                    need.append((b[0], True))
                for t in b[1]:
                    need.append((t, False))
        out = {}
        for (tok, true_dep) in need:
            sem, val, src = tok
            if src == eng:
                if eng in ("pe", "sp"):
                    continue
                if not true_dep:
                    continue
            if use_known and self.known[eng].get(sem.name, 0) >= val:
                continue
            if sem.name not in out or out[sem.name][1] < val:
                out[sem.name] = (sem, val)
        if use_known:
            for nm, (sem, val) in out.items():
                self.known[eng][nm] = val
        return list(out.values())

    def _record(self, tok, r, w):
        for k in r:
            b = self.buf.setdefault(k, [None, []])
            b[1].append(tok)
        for k in w:
            self.buf[k] = [tok, []]

    def op(self, eng, fn, r=(), w=(), inc=True):
        waits = self._deps(eng, r, w)
        if inc:
            if self.cnt[eng] >= 20000:
                self.epoch[eng] += 1
                self.sem[eng] = self.nc.alloc_semaphore(name=f"sem_{eng}_{self.epoch[eng]}")
                self.cnt[eng] = 0
            self.cnt[eng] += 1
            tok = (self.sem[eng], self.cnt[eng], eng)
            self.ops[eng].append((waits, fn, (self.sem[eng], 1)))
        else:
            tok = (self.sem[eng], self.cnt[eng] + 1, eng)
            self.ops[eng].append((waits, fn, None))
        self.last[eng] = tok
        self._record(tok, r, w)

    def dma(self, eng, out, in_, r=(), w=(), dkey=None, after=None):
        waits = self._deps(eng, r, w, use_known=(after is None))
        if dkey not in self.dsem or self.dsem[dkey][1] >= 16000:
            self.dsem_n = getattr(self, "dsem_n", 0) + 1
            old = self.dsem.get(dkey)
            if old is not None:
                self.old_dsems = getattr(self, "old_dsems", []) + [(old[0], old[1], "dma")]
            self.dsem[dkey] = [self.nc.alloc_semaphore(name=f"dsem_{dkey}_{self.dsem_n}"), 0]
        ds = self.dsem[dkey]
        ds[1] += 16
        tok = (ds[0], ds[1], "dma")
        entry = (waits, lambda e, o=out, i=in_: e.dma_start(out=o, in_=i), (ds[0], 16))
        lst = self.ops[eng]
        if after is None:
            lst.append(entry)
        else:
            idx = len(lst) - 1
            while idx >= 0 and lst[idx] is not after:
                idx -= 1
            assert idx >= 0
            lst.insert(idx + 1, entry)
        self._record(tok, r, w)
        return entry

    def barrier(self):
        toks = [t for t in self.last.values()] + [(d[0], d[1], "dma") for d in self.dsem.values()]
        for eng in ENG:
            waits = []
            for (sem, val, src) in toks:
                if src == eng and eng != "sp":
                    pass
                if self.known[eng].get(sem.name, 0) >= val:
                    continue
                self.known[eng][sem.name] = val
                waits.append((sem, val))
            if waits:
                self.ops[eng].append((waits, None, None))

    def emit(self):
        nc = self.nc
        me = self

        def run(name, e):
            for waits, fn, inc in me.ops[name]:
                for sem, val in waits:
                    e.wait_ge(sem, val)
                if fn is None:
                    continue
                ins = fn(e)
                if inc is not None:
                    ins.then_inc(inc[0], inc[1])

        with nc.Block() as block:
            @block.sync
            def _(e):
                run("sp", e)

            @block.scalar
            def _(e):
                run("act", e)

            @block.vector
            def _(e):
                run("dve", e)

            @block.gpsimd
            def _(e):
                run("pool", e)

            @block.tensor
            def _(e):
                run("pe", e)


class Arena:
    def __init__(self, base):
        self.base = base
        self.off = 0

    def _shape(self, v, shape):
        if len(shape) == 1:
            return v
        if len(shape) == 2:
            return v.rearrange("p (a b) -> p a b", a=shape[0])
        return v.rearrange("p (a b c) -> p a b c", a=shape[0], b=shape[1])

    def f32(self, *shape):
        n = int(np.prod(shape))
        v = self.base[:, self.off:self.off + n]
        self.off += n
        return self._shape(v, shape)

    def bf16(self, *shape):
        n = int(np.prod(shape))
        words = (n + 1) // 2
        v = self.base[:, self.off:self.off + words].bitcast(BF16)[:, 0:n]
        self.off += words
        return self._shape(v, shape)


def pv_layout(NE):
    items = [("lng", 16), ("lnb", 16), ("lo", 64), ("hgg", 16), ("cvb", 8), ("clg", 8), ("clb", 8),
             ("l1g", 16), ("l1b", 16), ("l2g", 16), ("l2b", 16), ("cvw", 8 * 31), ("b1", NE * 32),
             ("b2", NE * 16), ("brt", 1)]
    off = {}
    o = 0
    for k, n in items:
        off[k] = (o, n)
        o += n
    return off, o


def build(S, NE, debug=False):
    NB = S // 512
    NG = S // 1024
    nc = bass.Bass("TRN2", target_bir_lowering=False)
    PVO, PVN = pv_layout(NE)

    def din(name, shape):
        return nc.dram_tensor(name, list(shape), F32, kind="ExternalInput").ap()

    x = din("x", [S, D])
    pp = din("p", [S, 256])
    w_in = din("w_in", [D, 8 * D])
    w_hg = din("w_hg", [D, D])
    w_co = din("w_co", [1024, D])
    w_out = din("w_out", [D, D])
    w_rt = din("w_rt", [D, NE])
    w_e1 = din("w_e1", [NE, D, 2 * D])
    w_e2 = din("w_e2", [NE, D, D])
    w_pg = din("w_pg", [D, D])
    w_pp = din("w_pp", [256, D])
    pv_d = din("pv", [128, PVN])
    cst_d = din("cst", [128, 4 * 128 + 512])
    out = nc.dram_tensor("out", [S, D], F32, kind="ExternalOutput").ap()
    skind = "ExternalOutput" if debug else "Internal"
    obwd = nc.dram_tensor("obwd", [16, 128, S], F32, kind=skind).ap()
    hsc = nc.dram_tensor("hsc", [8, 128, S], F32, kind=skind).ap()
    x1sc = nc.dram_tensor("x1sc", [16, 128, S], F32, kind=skind).ap()

    ARENA_WORDS = 52500
    arena_t = nc.alloc_sbuf_tensor("arena", [128, ARENA_WORDS], F32)
    A = Arena(arena_t[:, :])
    PS = [nc.alloc_psum_tensor(f"ps{i}", [128, 512], F32)[:, :] for i in range(8)]
    Sc = Sched(nc)

    cst = A.f32(4 * 128 + 512)
    ident = cst[:, 0:128]
    ones = cst[:, 128:256]
    maskF = cst[:, 256:384]
    maskB = cst[:, 384:512]
    mreset = cst[:, 512:1024]
    pv = A.f32(PVN)
    lbs = A.f32(6, 16)
    wrt = A.f32(16, NE)
    CONST_END = A.off

    def pvs(name, c=None):
        o, n = PVO[name]
        if c is None:
            return pv[:, o:o + n]
        return pv[:, o + c:o + c + 1]

    Sc.dma("sp", cst, cst_d, w=["cst"], dkey="cst")
    Sc.dma("sp", pv, pv_d, w=["pv"], dkey="pv")
    Sc.dma("sp", wrt, w_rt.rearrange("(kc p) e -> p kc e", p=128), w=["wrt"], dkey="wrt")
    lo = pvs("lo")
    for d in range(2):
        l0 = lo[:, d * 32:d * 32 + 16]
        l1 = lo[:, d * 32 + 16:d * 32 + 32]
        tmpc = lbs[:, 3 * d + 2, :]
        Sc.op("dve", lambda e, o=tmpc, a=l0, b=l1: e.tensor_tensor(o, a, b, ALU.subtract), r=["pv"], w=[f"lbt{d}"])
        Sc.op("act", lambda e, o=lbs[:, 3 * d, :], i=tmpc: e.activation(o, i, AF.Sigmoid), r=[f"lbt{d}"], w=[f"lb{d}"])
        Sc.op("act", lambda e, o=lbs[:, 3 * d + 1, :], i=tmpc: e.activation(o, i, AF.Sigmoid, scale=-1.0),
              r=[f"lbt{d}"], w=[f"oml{d}"])
        Sc.op("dve", lambda e, o=tmpc, i=lbs[:, 3 * d + 1, :]: e.tensor_scalar(o, i, -1.0, None, ALU.mult),
              r=[f"oml{d}"], w=[f"lbt{d}"])
    LBK = ["lb0", "oml0", "lbt0", "lb1", "oml1", "lbt1"]

    XT = [A.f32(D) for _ in range(4)]
    XNT = A.f32(16, 512)
    XNB = A.bf16(16, 512)
    WS = [A.bf16(16, 256) for _ in range(2)]
    QS = A.bf16(16, 512)
    VT = A.bf16(4, D)
    TMP = [A.f32(512) for _ in range(8)]
    QT2 = [A.bf16(512) for _ in range(2)]
    KT = A.bf16(512)
    SCT2 = [A.bf16(4, 128) for _ in range(2)]
    KDT2 = [A.bf16(4, 128) for _ in range(2)]
    SF = A.f32(16, 128)
    SB2 = [A.bf16(128) for _ in range(2)]
    CIN = A.bf16(8, 512)
    HB = [A.f32(542) for _ in range(2)]
    OST = [A.f32(512) for _ in range(2)]
    STAT = A.f32(4, 32)
    ESM2 = [A.f32(2, 4) for _ in range(2)]
    RR = A.f32(512)
    MEANB = A.f32(512)
    assert A.off <= ARENA_WORDS, A.off
    ws_ctr = [0]

    ws_last = [None]

    def load_slab(src_ap, ncols=256, nk=16):
        i = ws_ctr[0] % 2
        ws_ctr[0] += 1
        key = f"ws{i}"
        dst = WS[i][:, 0:nk, 0:ncols]
        Sc.dma("pool", dst, src_ap.rearrange("(kc p) c -> p kc c", p=128), w=[key], dkey=key, after=ws_last[0])
        marker = ([], None, None)
        Sc.ops["pool"].append(marker)
        ws_last[0] = marker
        return WS[i], key

    def mm(out_, lhsT, rhs, start, stop, r, w, inc):
        Sc.op("pe", lambda e: e.matmul(out_, lhsT, rhs, start=start, stop=stop), r=r, w=w, inc=inc)

    def tr(out_, in_, r, w, inc, kp=128):
        idn = ident[0:kp, 0:kp]
        Sc.op("pe", lambda e: e.transpose(out_, in_, idn), r=list(r) + ["cst"], w=w, inc=inc)

    def act(out_, in_, func, r, w, bias=None, scale=None):
        kw = {}
        if bias is not None:
            kw["bias"] = bias
        if scale is not None:
            kw["scale"] = scale
        Sc.op("act", lambda e: e.activation(out_, in_, func, **kw), r=r, w=w)

    def ts(eng, out_, in0, s1, s2, op0, op1, r, w):
        if op1 is None:
            Sc.op(eng, lambda e: e.tensor_scalar(out_, in0, s1, None, op0), r=r, w=w)
        else:
            Sc.op(eng, lambda e: e.tensor_scalar(out_, in0, s1, s2, op0, op1), r=r, w=w)

    def tt(eng, out_, in0, in1, op, r, w):
        Sc.op(eng, lambda e: e.tensor_tensor(out_, in0, in1, op), r=r, w=w)

    def stt(eng, out_, in0, scalar, in1, op0, op1, r, w):
        Sc.op(eng, lambda e: e.scalar_tensor_tensor(out_, in0, scalar, in1, op0, op1), r=r, w=w)

    psrot = [0]

    def next_ps(lo_=0, n=2):
        i = lo_ + (psrot[0] % n)
        psrot[0] += 1
        return PS[i], f"ps{i}"

    def ln_rows(tile, key, tagi):
        st = TMP[7][:, 0:24].rearrange("p (a b) -> p a b", a=4)
        mv = STAT[:, tagi, 0:2]
        rs = STAT[:, tagi, 2:3]
        for c4 in range(4):
            Sc.op("dve", lambda e, o=st[:, c4, :], i=tile[:, c4 * 512:(c4 + 1) * 512]: e.bn_stats(o, i),
                  r=[key], w=["tmp7"])
        Sc.op("dve", lambda e: e.bn_aggr(mv, TMP[7][:, 0:24]), r=["tmp7"], w=[f"mv{tagi}"])
        ts("dve", rs, mv[:, 1:2], EPS, None, ALU.add, None, r=[f"mv{tagi}"], w=[f"rs{tagi}"])
        act(rs, rs, AF.Sqrt, r=[f"rs{tagi}"], w=[f"rs{tagi}"])
        Sc.op("dve", lambda e: e.reciprocal(rs, rs), r=[f"rs{tagi}"], w=[f"rs{tagi}"])
        ts("dve", tile, tile, mv[:, 0:1], rs, ALU.subtract, ALU.mult, r=[key, f"mv{tagi}", f"rs{tagi}"], w=[key])

    def front(j, want_f32):
        for i in range(4):
            Sc.dma("sp", XT[i], x[j * 512 + i * 128:j * 512 + (i + 1) * 128, :], w=[f"XT{i}"], dkey=f"xt{i}")
            ln_rows(XT[i], f"XT{i}", i)
        for c in range(16):
            ps, pk = next_ps(2, 2)
            for i in range(4):
                tr(ps[:, i * 128:(i + 1) * 128], XT[i][:, c * 128:(c + 1) * 128], r=[f"XT{i}"], w=[pk], inc=(i == 3))
            if want_f32:
                act(XNT[:, c, :], ps, AF.Identity, r=[pk, "pv"], w=[f"xnt{c}"], bias=pvs("lnb", c), scale=pvs("lng", c))
                Sc.op("pool", lambda e, o=XNB[:, c, :], i_=XNT[:, c, :]: e.tensor_copy(o, i_), r=[f"xnt{c}"], w=[f"xnb{c}"])
            else:
                act(XNB[:, c, :], ps, AF.Identity, r=[pk, "pv"], w=[f"xnb{c}"], bias=pvs("lnb", c), scale=pvs("lng", c))

    XNBK = [f"xnb{c}" for c in range(16)]

    def proj_fm(col0, consume):
        wsl, wk = load_slab(w_in[:, col0:col0 + 256])
        for mi in range(2):
            ps, pk = next_ps(0, 2)
            for k in range(16):
                mm(ps, wsl[:, k, mi * 128:(mi + 1) * 128], XNB[:, k, :], k == 0, k == 15,
                   r=[wk, XNBK[k]], w=[pk], inc=(k == 15))
            consume(mi, ps, pk)

    def proj_q():
        for sl in range(8):
            def cons(mi, ps, pk, sl=sl):
                h = sl * 2 + mi
                act(QS[:, h, :], ps, AF.Silu, r=[pk], w=[f"qs{h}"])
            proj_fm(0 * D + sl * 256, cons)

    def proj_v():
        for sl in range(8):
            wsl, wk = load_slab(w_in[:, 3 * D + sl * 256:3 * D + (sl + 1) * 256])
            for i in range(4):
                ps, pk = next_ps(0, 2)
                for k in range(16):
                    mm(ps[:, 0:256], XNB[:, k, i * 128:(i + 1) * 128], wsl[:, k, :], k == 0, k == 15,
                       r=[wk, XNBK[k]], w=[pk], inc=(k == 15))
                Sc.op("act", lambda e, o=VT[:, i, sl * 256:(sl + 1) * 256], p_=ps[:, 0:256]: e.copy(o, p_),
                      r=[pk], w=[f"vt{i}"])

    def scan_dir(j, d, emit_o):
        lb = lbs[:, 3 * d, :]
        oml = lbs[:, 3 * d + 1, :]
        noml = lbs[:, 3 * d + 2, :]
        fcol = (1 + d) * D
        chunks = [0, 1, 2, 3] if d == 0 else [3, 2, 1, 0]
        last = 127 if d == 0 else 0
        mask = maskF if d == 0 else maskB
        sg, g, kk, dd, ep, kd = TMP[0], TMP[1], TMP[2], TMP[4], TMP[5], TMP[1]
        bpre = TMP[3]
        if d == 0:
            bb, en, KBB, KEN = TMP[3], TMP[6], "tmp3", "tmp6"
        else:
            bb, en, KBB, KEN = TMP[6], TMP[3], "tmp6", "tmp3"
        bb3 = bb.rearrange("p (c t) -> p c t", c=4)
        bpre3 = bpre.rearrange("p (c t) -> p c t", c=4)
        dd3 = dd.rearrange("p (c t) -> p c t", c=4)
        ep3 = ep.rearrange("p (c t) -> p c t", c=4)
        kd3 = kd.rearrange("p (c t) -> p c t", c=4)
        OB = [(PS[6], "ps6"), (PS[3], "ps3")]
        PB = [(PS[7], "ps7"), (PS[2], "ps2")]
        for sl in range(8):
            wsl, wk = load_slab(w_in[:, fcol + sl * 256:fcol + (sl + 1) * 256])
            pss = []
            for mi in range(2):
                ps, pk = next_ps(0, 2)
                for k in range(16):
                    mm(ps, wsl[:, k, mi * 128:(mi + 1) * 128], XNB[:, k, :], k == 0, k == 15,
                       r=[wk, XNBK[k]], w=[pk], inc=(k == 15))
                pss.append((ps, pk))
            for mi in range(2):
                ps, pk = pss[mi]
                h = sl * 2 + mi
                QT, SCT, KDT, esm = QT2[mi], SCT2[mi], KDT2[mi], ESM2[mi]
                kq, ksc, kkd, kem, kel = f"qt{mi}", f"sct{mi}", f"kdt{mi}", f"emid{mi}", f"elast{mi}"
                act(sg, ps, AF.Sigmoid, r=[pk], w=["tmp0"])
                act(g, sg, AF.Ln, r=["tmp0"] + LBK, w=["tmp1"], bias=lb[:, h:h + 1], scale=oml[:, h:h + 1])
                ts("dve", kk, sg, noml[:, h:h + 1], oml[:, h:h + 1], ALU.mult, ALU.add, r=["tmp0"] + LBK, w=["tmp2"])
                Sc.op("dve", lambda e: e.tensor_tensor_scan(bpre, mreset, g, 0.0, ALU.mult, ALU.add),
                      r=["tmp1", "cst"], w=["tmp3"])
                if d == 1:
                    tt("dve", dd, g, bpre, ALU.subtract, r=["tmp1", "tmp3"], w=["tmp4"])
                    tt("dve", bb3, dd3, bpre3[:, :, 127:128].to_broadcast([128, 4, 128]), ALU.add,
                       r=["tmp4", "tmp3"], w=["tmp6"])
                act(esm[:, 0, :], bb3[:, :, 64], AF.Exp, r=[KBB], w=[kem])
                act(esm[:, 1, :], bb3[:, :, last], AF.Exp, r=[KBB], w=[kel])
                tt("dve", dd3, bb3, bb3[:, :, 64:65].to_broadcast([128, 4, 128]), ALU.subtract, r=[KBB], w=["tmp4"])
                act(ep, dd, AF.Exp, r=["tmp4"], w=["tmp5"])
                act(en, dd, AF.Exp, r=["tmp4"], w=[KEN], scale=-1.0)
                tt("pool", QT, QS[:, h, :], ep, ALU.mult, r=[f"qs{h}", "tmp5"], w=[kq])
                tt("dve", KT, kk, en, ALU.mult, r=["tmp2", KEN], w=["kt"])
                tt("dve", kd3, KT.rearrange("p (c t) -> p c t", c=4),
                   ep3[:, :, last:last + 1].to_broadcast([128, 4, 128]), ALU.mult, r=["kt", "tmp5"], w=["tmp1"])
                for c in range(4):
                    mm(PS[4][:, c * 128:(c + 1) * 128], KT[:, c * 128:(c + 1) * 128], QT[:, c * 128:(c + 1) * 128],
                       True, True, r=["kt", kq], w=["ps4"], inc=(c == 3))
                tt("dve", SCT, PS[4].rearrange("p (c t) -> p c t", c=4),
                   mask.rearrange("p (o t) -> p o t", o=1).to_broadcast([128, 4, 128]), ALU.mult,
                   r=["ps4", "cst"], w=[ksc])
                for c in range(4):
                    tr(PS[5][:, c * 128:(c + 1) * 128], kd[:, c * 128:(c + 1) * 128], r=["tmp1"], w=["ps5"], inc=(c == 3))
                Sc.op("act", lambda e, KDT=KDT: e.copy(KDT, PS[5].rearrange("p (c t) -> p c t", c=4)), r=["ps5"], w=[kkd])
            for c in chunks:
                for mi in range(2):
                    h = sl * 2 + mi
                    QT, SCT, KDT, esm, SB = QT2[mi], SCT2[mi], KDT2[mi], ESM2[mi], SB2[mi]
                    kq, ksc, kkd, kem, kel, ksb = f"qt{mi}", f"sct{mi}", f"kdt{mi}", f"emid{mi}", f"elast{mi}", f"sb{mi}"
                    ops, opk = OB[mi]
                    pps, ppk = PB[mi]
                    vk = VT[:, c, h * 128:(h + 1) * 128]
                    act(SB, SF[:, h, :], AF.Identity, r=[f"sf{h}", kem], w=[ksb], scale=esm[:, 0, c:c + 1])
                    mm(ops[:, c * 128:(c + 1) * 128], vk, SCT[:, c, :], True, False, r=[f"vt{c}", ksc], w=[opk], inc=False)
                    mm(ops[:, c * 128:(c + 1) * 128], SB, QT[:, c * 128:(c + 1) * 128], False, True,
                       r=[ksb, kq], w=[opk], inc=True)
                    mm(pps[:, 0:128], KDT[:, c, :], vk, True, True, r=[kkd, f"vt{c}"], w=[ppk], inc=True)
                    stt("dve", SF[:, h, :], SF[:, h, :], esm[:, 1, c:c + 1], pps[:, 0:128], ALU.mult, ALU.add,
                        r=[ppk, kel, f"sf{h}", ksb], w=[f"sf{h}"])
            for mi in range(2):
                emit_o(sl * 2 + mi, OB[mi][0], OB[mi][1])

    def reset_state():
        Sc.op("dve", lambda e: e.memset(SF, 0.0), r=[], w=[f"sf{h}" for h in range(16)])

    reset_state()
    for j in reversed(range(NB)):
        front(j, False)
        proj_q()
        proj_v()
        ostc = [0]

        def emit_o_A(h, ops, opk, j=j):
            i = ostc[0] % 2
            ostc[0] += 1
            Sc.op("act", lambda e: e.copy(OST[i], ops), r=[opk], w=[f"ost{i}"])
            Sc.dma("sp", obwd[h, :, j * 512:(j + 1) * 512], OST[i], r=[f"ost{i}"], w=[f"obwd{j}_{h}"], dkey=f"ost{i}")
        scan_dir(j, 1, emit_o_A)
        for sl in range(4):
            held = {}

            def cons_g(mi, ps, pk, sl=sl, held=held):
                t = TMP[mi]
                act(t, ps, AF.Sigmoid, r=[pk], w=[f"tmp{mi}"])
            proj_fm(5 * D + 1024 + sl * 256, cons_g)

            def cons_a(mi, ps, pk, sl=sl, j=j):
                i = ostc[0] % 2
                ostc[0] += 1
                ch = sl * 2 + mi
                tt("dve", OST[i], ps, TMP[mi], ALU.mult, r=[pk, f"tmp{mi}"], w=[f"ost{i}"])
                Sc.dma("sp", hsc[ch, :, j * 512:(j + 1) * 512], OST[i], r=[f"ost{i}"], w=[f"hsc{j}_{ch}"], dkey=f"ost{i}")
            proj_fm(5 * D + sl * 256, cons_a)
    Sc.barrier()
    ws_last[0] = None

    reset_state()
    CACC = TMP
    AIN = QS
    MIXB = VT.rearrange("p a b -> p (a b)").rearrange("p (a b) -> p a b", a=16)
    OBW = [XT[i].rearrange("p (a b) -> p a b", a=4) for i in range(4)]

    def fm_stats(srcs, keys, nfeat, sq, MEANB, RR, ksq, kmean, krr):
        n = len(srcs)
        for c in range(n):
            mm(PS[4], ones, srcs[c], c == 0, c == n - 1, r=[keys[c], "cst"], w=["ps4"], inc=(c == n - 1))
        for c in range(n):
            act(sq, srcs[c], AF.Square, r=[keys[c]], w=[ksq])
            mm(PS[5], ones, sq, c == 0, c == n - 1, r=[ksq, "cst"], w=["ps5"], inc=True)
        ts("dve", MEANB, PS[4], 1.0 / nfeat, None, ALU.mult, None, r=["ps4"], w=[kmean])
        tt("dve", RR, MEANB, MEANB, ALU.mult, r=[kmean], w=[krr])
        stt("dve", RR, PS[5], 1.0 / nfeat, RR, ALU.mult, ALU.subtract, r=["ps5", krr], w=[krr])
        ts("dve", RR, RR, EPS, None, ALU.add, None, r=[krr], w=[krr])
        act(RR, RR, AF.Sqrt, r=[krr], w=[krr])
        Sc.op("dve", lambda e: e.reciprocal(RR, RR), r=[krr], w=[krr])

    for j in range(NB):
        front(j, True)
        t0 = j * 512 - 15
        for ch in range(8):
            hb = HB[ch % 2]
            hk = f"hb{ch % 2}"
            lo_t = max(t0, 0)
            hi_t = min(t0 + 542, S)
            if lo_t > t0 or hi_t < t0 + 542:
                Sc.op("pool", lambda e, hb=hb: e.memset(hb, 0.0), r=[], w=[hk])
            Sc.dma("sp", hb[:, lo_t - t0:hi_t - t0], hsc[ch, :, lo_t:hi_t],
                   r=[f"hsc{jj}_{ch}" for jj in range(max(j - 1, 0), min(j + 2, NB))], w=[hk], dkey=hk)
            cw_o = PVO["cvw"][0] + ch * 31
            acc = CACC[ch]
            ts("dve", acc, hb[:, 0:512], pv[:, cw_o:cw_o + 1], pvs("cvb", ch), ALU.mult, ALU.add,
               r=[hk, "pv"], w=[f"tmp{ch}"])
            for k in range(1, 31):
                Sc.op("dve", lambda e, acc=acc, hb=hb, k=k, cw_o=cw_o: e.scalar_tensor_tensor(
                    acc, hb[:, k:k + 512], pv[:, cw_o + k:cw_o + k + 1], acc, ALU.mult, ALU.add),
                    r=[hk] if k < 30 else [hk], w=[f"tmp{ch}"])
        fm_stats([CACC[c] for c in range(8)], [f"tmp{c}" for c in range(8)], 1024.0, OST[0], MEANB, RR, "ost0", "meanb", "rr")
        for ch in range(8):
            tt("dve", CACC[ch], CACC[ch], MEANB, ALU.subtract, r=["meanb"], w=[f"tmp{ch}"])
            tt("dve", CACC[ch], CACC[ch], RR, ALU.mult, r=["rr"], w=[f"tmp{ch}"])
            act(CIN[:, ch, :], CACC[ch], AF.Silu, r=[f"tmp{ch}", "pv"], w=[f"cin{ch}"],
                bias=pvs("clb", ch), scale=pvs("clg", ch))
        proj_q()
        proj_v()
        for i in range(4):
            Sc.dma("sp", OBW[i], obwd[4 * i:4 * i + 4, :, j * 512:(j + 1) * 512].rearrange("h p s -> p h s"),
                   r=[f"obwd{j}_{h}" for h in range(4 * i, 4 * i + 4)], w=[f"XT{i}"], dkey=f"xt{i}")

        def emit_o_B(h, ops, opk):
            tt("dve", OBW[h // 4][:, h % 4, :], ops, OBW[h // 4][:, h % 4, :], ALU.add, r=[opk], w=[f"XT{h // 4}"])
        scan_dir(j, 0, emit_o_B)
        sq = OST[0]
        for h in range(16):
            act(sq, OBW[h // 4][:, h % 4, :], AF.Square, r=[f"XT{h // 4}"], w=["ost0"])
            mm(PS[5], ones, sq, h == 0, h == 15, r=["ost0", "cst"], w=["ps5"], inc=True)
        ts("dve", RR, PS[5], 1.0 / D, EPS, ALU.mult, ALU.add, r=["ps5"], w=["rr"])
        act(RR, RR, AF.Sqrt, r=["rr"], w=["rr"])
        Sc.op("dve", lambda e: e.reciprocal(RR, RR), r=["rr"], w=["rr"])
        for sl in range(8):
            def cons_og(mi, ps, pk, sl=sl):
                h = sl * 2 + mi
                t = TMP[mi]
                act(t, ps, AF.Silu, r=[pk], w=[f"tmp{mi}"])
                tt("dve", TMP[2 + mi], OBW[h // 4][:, h % 4, :], RR, ALU.mult, r=[f"XT{h // 4}", "rr"], w=[f"tmp{2 + mi}"])
                stt("dve", AIN[:, h, :], t, pvs("hgg", h), TMP[2 + mi], ALU.mult, ALU.mult,
                    r=[f"tmp{mi}", f"tmp{2 + mi}", "pv"], w=[f"qs{h}"])
            proj_fm(4 * D + sl * 256, cons_og)
        AINK = [f"qs{h}" for h in range(16)]
        CINK = [f"cin{c}" for c in range(8)]
        for sl in range(8):
            def cons_ga(mi, ps, pk):
                act(TMP[mi], ps, AF.Sigmoid, r=[pk], w=[f"tmp{mi}"])
            proj_fm(6 * D + sl * 256, cons_ga)

            def cons_gc(mi, ps, pk):
                act(TMP[2 + mi], ps, AF.Sigmoid, r=[pk], w=[f"tmp{2 + mi}"])
            proj_fm(7 * D + sl * 256, cons_gc)
            wsl, wk = load_slab(w_hg[:, sl * 256:(sl + 1) * 256])
            for mi in range(2):
                ps, pk = next_ps(0, 2)
                for k in range(16):
                    mm(ps, wsl[:, k, mi * 128:(mi + 1) * 128], AIN[:, k, :], k == 0, k == 15, r=[wk, AINK[k]], w=[pk], inc=(k == 15))
                tt("dve", TMP[mi], ps, TMP[mi], ALU.mult, r=[pk], w=[f"tmp{mi}"])
            wsl, wk = load_slab(w_co[:, sl * 256:(sl + 1) * 256], nk=8)
            for mi in range(2):
                m = sl * 2 + mi
                ps, pk = next_ps(0, 2)
                for k in range(8):
                    mm(ps, wsl[:, k, mi * 128:(mi + 1) * 128], CIN[:, k, :], k == 0, k == 7, r=[wk, CINK[k]], w=[pk], inc=(k == 7))
                tt("dve", TMP[2 + mi], ps, TMP[2 + mi], ALU.mult, r=[pk], w=[f"tmp{2 + mi}"])
                tt("pool", MIXB[:, m, :], TMP[mi], TMP[2 + mi], ALU.add, r=[f"tmp{mi}", f"tmp{2 + mi}"], w=[f"vt{m // 4}"])
        for sl in range(8):
            wsl, wk = load_slab(w_out[:, sl * 256:(sl + 1) * 256])
            for mi in range(2):
                m = sl * 2 + mi
                ps, pk = next_ps(0, 2)
                for k in range(16):
                    mm(ps, wsl[:, k, mi * 128:(mi + 1) * 128], MIXB[:, k, :], k == 0, k == 15,
                       r=[wk, f"vt{k // 4}"], w=[pk], inc=(k == 15))
                stt("dve", XNT[:, m, :], XNT[:, m, :], ALPHA, ps, ALU.mult, ALU.add, r=[pk], w=[f"xnt{m}"])
        fm_stats([XNT[:, c, :] for c in range(16)], [f"xnt{c}" for c in range(16)], float(D), OST[0], MEANB, RR, "ost0", "meanb", "rr")
        for m in range(16):
            tt("dve", XNT[:, m, :], XNT[:, m, :], MEANB, ALU.subtract, r=["meanb"], w=[f"xnt{m}"])
            tt("dve", XNT[:, m, :], XNT[:, m, :], RR, ALU.mult, r=["rr"], w=[f"xnt{m}"])
            act(XNT[:, m, :], XNT[:, m, :], AF.Identity, r=[f"xnt{m}", "pv"], w=[f"xnt{m}"],
                bias=pvs("l1b", m), scale=pvs("l1g", m))
        Sc.dma("sp", x1sc[:, :, j * 512:(j + 1) * 512].rearrange("c p s -> p c s"), XNT,
               r=[f"xnt{m}" for m in range(16)], w=[f"x1sc{j}"], dkey="x1st")
    Sc.barrier()

    A.off = CONST_END
    ACC = A.f32(16, 1024)
    X1B = A.bf16(16, 1024)
    actb_off = A.off
    ACTB = A.bf16(16, 1024)
    end_actb = A.off
    A.off = actb_off
    WPP = A.bf16(2, D)
    PT = A.bf16(2, 1024)
    assert A.off <= end_actb
    A.off = end_actb
    WB = [A.bf16(16, 256) for _ in range(3)]
    GBC = A.f32(1024)
    T = [A.f32(512) for _ in range(6)]
    ZT = A.f32(D)
    LG = A.f32(8, NE)
    GT = A.f32(1024)
    GTE = A.f32(1024)
    SM = A.f32(8, 16)
    assert A.off <= ARENA_WORDS, A.off
    ACTK = [f"actb{i}" for i in range(16)]
    wc = [0, 0]

    for gi in range(NG):
        tok0 = gi * 1024
        Sc.dma("sp", ACC, x1sc[:, :, tok0:tok0 + 1024].rearrange("c p s -> p c s"),
               r=[f"x1sc{2 * gi}", f"x1sc{2 * gi + 1}"], w=[f"acc{m}" for m in range(16)], dkey="accld")
        for m in range(16):
            Sc.op("pool", lambda e, m=m: e.tensor_copy(X1B[:, m, :], ACC[:, m, :]), r=[f"acc{m}"], w=[f"x1b{m}"])
        for blk in range(2):
            ps, pk = next_ps(6, 1)
            for k in range(16):
                mm(ps[0:NE, :], wrt[:, k, :], ACC[:, k, blk * 512:(blk + 1) * 512], k == 0, k == 15,
                   r=["wrt", f"acc{k}"], w=[pk], inc=(k == 15))
            ts("dve", GT[0:NE, blk * 512:(blk + 1) * 512], ps[0:NE, :], pv[0:NE, PVO["brt"][0]:PVO["brt"][0] + 1], None,
               ALU.add, None, r=[pk, "pv"], w=["gt"])
        for t8 in range(8):
            ps, pk = next_ps(7, 1)
            tr(ps[:, 0:NE], GT[0:NE, t8 * 128:(t8 + 1) * 128], r=["gt"], w=[pk], inc=True, kp=NE)
            lg = LG[:, t8, :]
            sm = SM[:, t8, :]
            lk = f"lg{t8}"
            sk = f"sm{t8}"
            Sc.op("dve", lambda e, lg=lg, ps=ps: e.tensor_copy(lg, ps[:, 0:NE]), r=[pk], w=[lk])
            Sc.op("dve", lambda e, lg=lg, sm=sm: e.max(sm[:, 0:8], lg), r=[lk], w=[sk])
            ts("dve", sm[:, 8:9], sm[:, 0:1], -1.0, None, ALU.mult, None, r=[sk], w=[sk])
            ts("dve", T[0][:, 0:NE], lg, sm[:, 3:4], None, ALU.is_ge, None, r=[lk, sk], w=["t0"])
            act(lg, lg, AF.Exp, r=[lk, sk], w=[lk], bias=sm[:, 8:9])
            tt("dve", lg, lg, T[0][:, 0:NE], ALU.mult, r=["t0", lk], w=[lk])
            Sc.op("dve", lambda e, lg=lg, sm=sm: e.reduce_sum(sm[:, 9:10], lg, mybir.AxisListType.X),
                  r=[lk], w=[sk])
            Sc.op("dve", lambda e, sm=sm: e.reciprocal(sm[:, 9:10], sm[:, 9:10]), r=[sk], w=[sk])
            ts("dve", lg, lg, sm[:, 9:10], None, ALU.mult, None, r=[sk, lk], w=[lk])
        for t8 in range(8):
            ps, pk = next_ps(7, 1)
            tr(ps[0:NE, 0:128], LG[:, t8, :], r=[f"lg{t8}"], w=[pk], inc=True)
            Sc.op("act", lambda e, t8=t8, ps=ps: e.copy(GT[0:NE, t8 * 128:(t8 + 1) * 128], ps[0:NE, 0:128]),
                  r=[pk], w=["gt"])
        Sc.dma("pool", WPP, w_pp.rearrange("(kc p) c -> p kc c", p=128), w=ACTK, dkey="wpp")
        for t8 in range(8):
            zt = ZT[:, 0:256]
            Sc.dma("sp", zt, pp[tok0 + t8 * 128:tok0 + (t8 + 1) * 128, :], w=["zt"], dkey="zt")
            for k in range(2):
                ps, pk = next_ps(7, 1)
                tr(ps[:, 0:128], zt[:, k * 128:(k + 1) * 128], r=["zt"], w=[pk], inc=True)
                Sc.op("act", lambda e, k=k, t8=t8, ps=ps: e.copy(PT[:, k, t8 * 128:(t8 + 1) * 128], ps[:, 0:128]),
                      r=[pk] + ACTK, w=["pt"])
        b1o = PVO["b1"][0]
        b2o = PVO["b2"][0]
        items = [("pg", sl) for sl in range(8)]
        for ex in range(NE):
            items += [("w1", ex, i) for i in range(16)] + [("w2", ex, s2) for s2 in range(8)]

        def load_item(it):
            wi = wc[0] % 3
            wc[0] += 1
            key = f"wb{wi}"
            rr_ = lambda ap: ap.rearrange("(kc p) c -> p kc c", p=128)
            if it[0] == "pg":
                Sc.dma("pool", WB[wi], rr_(w_pg[:, it[1] * 256:(it[1] + 1) * 256]), w=[key], dkey=key)
            elif it[0] == "w1":
                _, ex, i = it
                Sc.dma("pool", WB[wi][:, :, 0:128], rr_(w_e1[ex, :, i * 128:(i + 1) * 128]), w=[key], dkey=key)
                Sc.dma("pool", WB[wi][:, :, 128:256], rr_(w_e1[ex, :, D + i * 128:D + (i + 1) * 128]), w=[key], dkey=key)
            else:
                _, ex, s2 = it
                Sc.dma("pool", WB[wi], rr_(w_e2[ex, :, s2 * 256:(s2 + 1) * 256]), w=[key], dkey=key)
            return WB[wi], key

        def compute_item(it, wt, wk):
            if it[0] == "pg":
                sl = it[1]
                for mi in range(2):
                    m = sl * 2 + mi
                    for blk in range(2):
                        bs = slice(blk * 512, (blk + 1) * 512)
                        ps, pk = next_ps(0, 2)
                        for k in range(16):
                            mm(ps, wt[:, k, mi * 128:(mi + 1) * 128], X1B[:, k, bs], k == 0, k == 15,
                               r=[wk, f"x1b{k}"], w=[pk], inc=(k == 15))
                        act(T[0], ps, AF.Sigmoid, r=[pk], w=["t0"])
                        ps2, pk2 = next_ps(2, 2)
                        for k in range(2):
                            mm(ps2, WPP[:, k, m * 128:(m + 1) * 128], PT[:, k, bs], k == 0, k == 1,
                               r=ACTK + ["pt"], w=[pk2], inc=(k == 1))
                        tt("dve", T[0], T[0], ps2, ALU.mult, r=[pk2, "t0"], w=["t0"])
                        stt("dve", ACC[:, m, bs], ACC[:, m, bs], ALPHA, T[0], ALU.mult, ALU.add,
                            r=["t0", f"x1b{m}", f"acc{m}"], w=[f"acc{m}"])
            elif it[0] == "w1":
                _, ex, i = it
                if i == 0:
                    ts("dve", GTE[0:NE, :], GT[0:NE, :], ident[0:NE, ex:ex + 1], None, ALU.mult, None,
                       r=["gt", "cst"], w=["gte"])
                    for blk in range(2):
                        bs = slice(blk * 512, (blk + 1) * 512)
                        ps, pk = next_ps(6, 1)
                        mm(ps, ones[0:NE, :], GTE[0:NE, bs], True, True, r=["cst", "gte"], w=[pk], inc=True)
                        Sc.op("act", lambda e, ps=ps, bs=bs: e.copy(GBC[:, bs], ps), r=[pk], w=[f"gbc{blk}"])
                for blk in range(2):
                    bs = slice(blk * 512, (blk + 1) * 512)
                    psg, pkg = next_ps(0, 2)
                    for k in range(16):
                        mm(psg, wt[:, k, 0:128], X1B[:, k, bs], k == 0, k == 15, r=[wk, f"x1b{k}"], w=[pkg], inc=(k == 15))
                    psl, pkl = next_ps(2, 2)
                    for k in range(16):
                        mm(psl, wt[:, k, 128:256], X1B[:, k, bs], k == 0, k == 15, r=[wk, f"x1b{k}"], w=[pkl], inc=(k == 15))
                    bg = pv[:, b1o + ex * 32 + i:b1o + ex * 32 + i + 1]
                    bl = pv[:, b1o + ex * 32 + 16 + i:b1o + ex * 32 + 16 + i + 1]
                    ts("dve", T[1], psg, bg, 7.0, ALU.add, ALU.min, r=[pkg, "pv"], w=["t1"])
                    act(T[2], T[1], AF.Sigmoid, r=["t1"], w=["t2"], scale=1.702)
                    act(T[3], psl, AF.Identity, r=[pkl, "pv"], w=["t3"], bias=bl)
                    ts("dve", T[3], T[3], 7.0, -7.0, ALU.min, ALU.max, r=["t3"], w=["t3"])
                    tt("dve", T[1], T[1], T[2], ALU.mult, r=["t1", "t2"], w=["t1"])
                    stt("dve", ACTB[:, i, bs], T[3], 1.0, T[1], ALU.add, ALU.mult, r=["t1", "t3", "pt"], w=[f"actb{i}"])
            else:
                _, ex, s2 = it
                for mi in range(2):
                    m = s2 * 2 + mi
                    for blk in range(2):
                        bs = slice(blk * 512, (blk + 1) * 512)
                        ps, pk = next_ps(4, 2)
                        for k in range(16):
                            mm(ps, wt[:, k, mi * 128:(mi + 1) * 128], ACTB[:, k, bs], k == 0, k == 15,
                               r=[wk, f"actb{k}"], w=[pk], inc=(k == 15))
                        b2 = pv[:, b2o + ex * 16 + m:b2o + ex * 16 + m + 1]
                        stt("dve", T[4 + blk], ps, b2, GBC[:, bs], ALU.add, ALU.mult, r=[pk, "pv", f"gbc{blk}"], w=[f"t{4 + blk}"])
                        tt("dve", ACC[:, m, bs], ACC[:, m, bs], T[4 + blk], ALU.add, r=[f"t{4 + blk}", f"acc{m}"], w=[f"acc{m}"])

        loaded = [load_item(items[0]), load_item(items[1])]
        for n, it in enumerate(items):
            if n + 2 < len(items):
                loaded.append(load_item(items[n + 2]))
            wt, wk = loaded[n]
            compute_item(it, wt, wk)
        for blk in range(2):
            bs = slice(blk * 512, (blk + 1) * 512)
            fm_stats([ACC[:, m, bs] for m in range(16)], [f"acc{m}" for m in range(16)], float(D),
                     T[0], T[1], T[2], "t0", "t1", "t2")
            for m in range(16):
                tt("dve", ACC[:, m, bs], ACC[:, m, bs], T[1], ALU.subtract, r=["t1", f"acc{m}"], w=[f"acc{m}"])
                tt("pool", ACC[:, m, bs], ACC[:, m, bs], T[2], ALU.mult, r=["t2", f"acc{m}"], w=[f"acc{m}"])
                act(ACC[:, m, bs], ACC[:, m, bs], AF.Identity, r=[f"acc{m}", "pv"], w=[f"acc{m}"],
                    bias=pvs("l2b", m), scale=pvs("l2g", m))
            for i in range(4):
                c0 = blk * 512 + i * 128
                for q4 in range(4):
                    ps, pk = next_ps(0, 4)
                    for mm_ in range(4):
                        m = q4 * 4 + mm_
                        tr(ps[:, mm_ * 128:(mm_ + 1) * 128], ACC[:, m, c0:c0 + 128], r=[f"acc{m}"], w=[pk], inc=(mm_ == 3))
                    Sc.op("act", lambda e, ps=ps, q4=q4: e.copy(ZT[:, q4 * 512:(q4 + 1) * 512], ps), r=[pk], w=["zt"])
                Sc.dma("sp", out[tok0 + c0:tok0 + c0 + 128, :], ZT, r=["zt"], w=["outst"], dkey="outst")
    Sc.barrier()
    Sc.emit()
    return nc


def _v16(v):
    return np.ascontiguousarray(np.asarray(v, np.float32).reshape(-1, 128).T)


def _prep_shared(inp, NE):
    PVO, PVN = pv_layout(NE)
    pv = np.zeros((128, PVN), np.float32)

    def put(name, a, c0=0):
        o, n = PVO[name]
        pv[:, o + c0:o + c0 + a.shape[1]] = a

    put("lng", _v16(inp["ln_emb_g"]))
    put("lnb", _v16(inp["ln_emb_b"]))
    lbd = np.asarray(inp["lower_bounds"], np.float32)
    for d in range(2):
        for sl in range(2):
            put("lo", _v16(lbd[d, sl]), d * 32 + sl * 16)
    put("hgg", _v16(inp["hg_norm_g"][0]))
    put("cvb", _v16(inp["conv_b"][0]))
    put("clg", _v16(inp["conv_ln_g"][0]))
    put("clb", _v16(inp["conv_ln_b"][0]))
    put("l1g", _v16(inp["ln1_g"][0]))
    put("l1b", _v16(inp["ln1_b"][0]))
    put("l2g", _v16(inp["ln2_g"][0]))
    put("l2b", _v16(inp["ln2_b"][0]))
    cw = np.asarray(inp["conv_w"][0], np.float32)
    put("cvw", np.ascontiguousarray(cw.T.reshape(8, 128, 31).transpose(1, 0, 2)).reshape(128, 8 * 31))
    b1 = np.asarray(inp["b_exp1"][0], np.float32)[:NE]
    put("b1", np.ascontiguousarray(b1.reshape(NE, 32, 128).transpose(2, 0, 1)).reshape(128, NE * 32))
    b2 = np.asarray(inp["b_exp2"][0], np.float32)[:NE]
    put("b2", np.ascontiguousarray(b2.reshape(NE, 16, 128).transpose(2, 0, 1)).reshape(128, NE * 16))
    o = PVO["brt"][0]
    pv[0:NE, o] = np.asarray(inp["b_router"][0], np.float32)[:NE]
    cst = np.zeros((128, 1024), np.float32)
    cst[:, 0:128] = np.eye(128, dtype=np.float32)
    cst[:, 128:256] = 1.0
    ii = np.arange(128)
    cst[:, 256:384] = (ii[:, None] <= ii[None, :]).astype(np.float32)
    cst[:, 384:512] = (ii[:, None] >= ii[None, :]).astype(np.float32)
    mr = np.ones(512, np.float32)
    mr[::128] = 0.0
    cst[:, 512:1024] = mr[None, :]
    sh = {
        "w_in": np.ascontiguousarray(inp["w_in"][0], np.float32),
        "w_hg": np.ascontiguousarray(inp["w_hg_out"][0], np.float32),
        "w_co": np.ascontiguousarray(inp["w_conv_out"][0], np.float32),
        "w_out": np.ascontiguousarray(inp["w_out"][0], np.float32),
        "w_rt": np.ascontiguousarray(inp["w_router"][0][:, :NE], np.float32),
        "w_e1": np.ascontiguousarray(inp["w_exp1"][0][:NE], np.float32),
        "w_e2": np.ascontiguousarray(inp["w_exp2"][0][:NE], np.float32),
        "w_pg": np.ascontiguousarray(inp["w_ple_gate"][0], np.float32),
        "w_pp": np.ascontiguousarray(inp["w_ple_proj"][0], np.float32),
        "pv": pv,
        "cst": cst,
    }
    return sh


_NC_CACHE = {}


def kernel(**inputs):
    S, NE = 4096, 32
    inp = {k: np.asarray(v) for k, v in inputs.items()}
    sh = _prep_shared(inp, NE)
    seqs = [(inp["x_prompt"][b], inp["p_prompt"][0, b]) for b in range(2)]
    seqs += [(inp["x_sample"][b], inp["p_sample"][0, b]) for b in range(4)]
    seqs += [seqs[0], seqs[1]]
    if (S, NE) not in _NC_CACHE:
        _NC_CACHE[(S, NE)] = build(S, NE)
    nc = _NC_CACHE[(S, NE)]
    in_maps = []
    for c in range(8):
        m = dict(sh)
        m["x"] = np.ascontiguousarray(seqs[c][0], np.float32)
        m["p"] = np.ascontiguousarray(seqs[c][1], np.float32)
        in_maps.append(m)
    res = run_bass_kernel_spmd(nc, in_maps, core_ids=list(range(8)))
    outs = [np.asarray(r["out"], np.float32) for r in res.results]
    y_prompt = np.stack(outs[0:2], axis=0)
    y_sample = np.stack(outs[2:6], axis=0)
    return (y_prompt, y_sample)
```

```python
import numpy as np
import concourse.bass as bass
import concourse.mybir as mybir
from concourse.bass_utils import run_bass_kernel_spmd

F32 = mybir.dt.float32
BF16 = mybir.dt.bfloat16
AF = mybir.ActivationFunctionType
ALU = mybir.AluOpType

D = 2048
NH = 16
EPS = 1e-5
ALPHA = 2.0 ** 0.25
TOPK = 4
ENG = ("sp", "act", "dve", "pool", "pe")


class Sched:
    def __init__(self, nc):
        self.nc = nc
        self.ops = {e: [] for e in ENG}
        self.cnt = {e: 0 for e in ENG}
        self.epoch = {e: 0 for e in ENG}
        self.sem = {e: nc.alloc_semaphore(name=f"sem_{e}_0") for e in ENG}
        self.known = {e: {} for e in ENG}
        self.buf = {}
        self.dsem = {}
        self.last = {}

    def _deps(self, eng, r, w, use_known=True):
        need = []
        for k in r:
            b = self.buf.get(k)
            if b and b[0]:
                need.append((b[0], True))
        for k in w:
            b = self.buf.get(k)
            if b:
                if b[0]:
                    need.append((b[0], True))
                for t in b[1]:
                    need.append((t, False))
        out = {}
        for (tok, true_dep) in need:
            sem, val, src = tok
            if src == eng:
                if eng in ("pe", "sp"):
                    continue
                if not true_dep:
                    continue
            if use_known and self.known[eng].get(sem.name, 0) >= val:
                continue
            if sem.name not in out or out[sem.name][1] < val:
                out[sem.name] = (sem, val)
        if use_known:
            for nm, (sem, val) in out.items():
                self.known[eng][nm] = val
        return list(out.values())

    def _record(self, tok, r, w):
        for k in r:
            b = self.buf.setdefault(k, [None, []])
            b[1].append(tok)
        for k in w:
            self.buf[k] = [tok, []]

    def op(self, eng, fn, r=(), w=(), inc=True):
        waits = self._deps(eng, r, w)
        if inc:
            if self.cnt[eng] >= 20000:
                self.epoch[eng] += 1
                self.sem[eng] = self.nc.alloc_semaphore(name=f"sem_{eng}_{self.epoch[eng]}")
                self.cnt[eng] = 0
            self.cnt[eng] += 1
            tok = (self.sem[eng], self.cnt[eng], eng)
            self.ops[eng].append((waits, fn, (self.sem[eng], 1)))
        else:
            tok = (self.sem[eng], self.cnt[eng] + 1, eng)
            self.ops[eng].append((waits, fn, None))
        self.last[eng] = tok
        self._record(tok, r, w)

    def dma(self, eng, out, in_, r=(), w=(), dkey=None, after=None):
        waits = self._deps(eng, r, w, use_known=(after is None))
        if dkey not in self.dsem or self.dsem[dkey][1] >= 16000:
            self.dsem_n = getattr(self, "dsem_n", 0) + 1
            old = self.dsem.get(dkey)
            if old is not None:
                self.old_dsems = getattr(self, "old_dsems", []) + [(old[0], old[1], "dma")]
            self.dsem[dkey] = [self.nc.alloc_semaphore(name=f"dsem_{dkey}_{self.dsem_n}"), 0]
        ds = self.dsem[dkey]
        ds[1] += 16
        tok = (ds[0], ds[1], "dma")
        entry = (waits, lambda e, o=out, i=in_: e.dma_start(out=o, in_=i), (ds[0], 16))
        lst = self.ops[eng]
        if after is None:
            lst.append(entry)
        else:
            idx = len(lst) - 1
            while idx >= 0 and lst[idx] is not after:
                idx -= 1
            assert idx >= 0
            lst.insert(idx + 1, entry)
        self._record(tok, r, w)
        return entry

    def barrier(self):
        toks = [t for t in self.last.values()] + [(d[0], d[1], "dma") for d in self.dsem.values()]
        for eng in ENG:
            waits = []
            for (sem, val, src) in toks:
                if src == eng and eng != "sp":
                    pass
                if self.known[eng].get(sem.name, 0) >= val:
                    continue
                self.known[eng][sem.name] = val
                waits.append((sem, val))
            if waits:
                self.ops[eng].append((waits, None, None))

    def emit(self):
        nc = self.nc
        me = self

        def run(name, e):
            for waits, fn, inc in me.ops[name]:
                for sem, val in waits:
                    e.wait_ge(sem, val)
                if fn is None:
                    continue
                ins = fn(e)
                if inc is not None:
                    ins.then_inc(inc[0], inc[1])

        with nc.Block() as block:
            @block.sync
            def _(e):
                run("sp", e)

            @block.scalar
            def _(e):
                run("act", e)

            @block.vector
            def _(e):
                run("dve", e)

            @block.gpsimd
            def _(e):
                run("pool", e)

            @block.tensor
            def _(e):
                run("pe", e)


class Arena:
    def __init__(self, base):
        self.base = base
        self.off = 0

    def _shape(self, v, shape):
        if len(shape) == 1:
            return v
        if len(shape) == 2:
            return v.rearrange("p (a b) -> p a b", a=shape[0])
        return v.rearrange("p (a b c) -> p a b c", a=shape[0], b=shape[1])

    def f32(self, *shape):
        n = int(np.prod(shape))
        v = self.base[:, self.off:self.off + n]
        self.off += n
        return self._shape(v, shape)

    def bf16(self, *shape):
        n = int(np.prod(shape))
        words = (n + 1) // 2
        v = self.base[:, self.off:self.off + words].bitcast(BF16)[:, 0:n]
        self.off += words
        return self._shape(v, shape)


def pv_layout(NE):
    items = [("lng", 16), ("lnb", 16), ("lo", 64), ("hgg", 16), ("cvb", 8), ("clg", 8), ("clb", 8),
             ("l1g", 16), ("l1b", 16), ("l2g", 16), ("l2b", 16), ("cvw", 8 * 31), ("b1", NE * 32),
             ("b2", NE * 16), ("brt", 1)]
    off = {}
    o = 0
    for k, n in items:
        off[k] = (o, n)
        o += n
    return off, o


def build(S, NE, debug=False):
    NB = S // 512
    NG = S // 1024
    nc = bass.Bass("TRN2", target_bir_lowering=False)
    PVO, PVN = pv_layout(NE)

    def din(name, shape):
        return nc.dram_tensor(name, list(shape), F32, kind="ExternalInput").ap()

    x = din("x", [S, D])
    pp = din("p", [S, 256])
    w_in = din("w_in", [D, 8 * D])
    w_hg = din("w_hg", [D, D])
    w_co = din("w_co", [1024, D])
    w_out = din("w_out", [D, D])
    w_rt = din("w_rt", [D, NE])
    w_e1 = din("w_e1", [NE, D, 2 * D])
    w_e2 = din("w_e2", [NE, D, D])
    w_pg = din("w_pg", [D, D])
    w_pp = din("w_pp", [256, D])
    pv_d = din("pv", [128, PVN])
    cst_d = din("cst", [128, 4 * 128 + 512])
    out = nc.dram_tensor("out", [S, D], F32, kind="ExternalOutput").ap()
    skind = "ExternalOutput" if debug else "Internal"
    obwd = nc.dram_tensor("obwd", [16, 128, S], F32, kind=skind).ap()
    hsc = nc.dram_tensor("hsc", [8, 128, S], F32, kind=skind).ap()
    x1sc = nc.dram_tensor("x1sc", [16, 128, S], F32, kind=skind).ap()

    ARENA_WORDS = 52500
    arena_t = nc.alloc_sbuf_tensor("arena", [128, ARENA_WORDS], F32)
    A = Arena(arena_t[:, :])
    PS = [nc.alloc_psum_tensor(f"ps{i}", [128, 512], F32)[:, :] for i in range(8)]
    Sc = Sched(nc)

    cst = A.f32(4 * 128 + 512)
    ident = cst[:, 0:128]
    ones = cst[:, 128:256]
    maskF = cst[:, 256:384]
    maskB = cst[:, 384:512]
    mreset = cst[:, 512:1024]
    pv = A.f32(PVN)
    lbs = A.f32(6, 16)
    wrt = A.f32(16, NE)
    CONST_END = A.off

    def pvs(name, c=None):
        o, n = PVO[name]
        if c is None:
            return pv[:, o:o + n]
        return pv[:, o + c:o + c + 1]

    Sc.dma("sp", cst, cst_d, w=["cst"], dkey="cst")
    Sc.dma("sp", pv, pv_d, w=["pv"], dkey="pv")
    Sc.dma("sp", wrt, w_rt.rearrange("(kc p) e -> p kc e", p=128), w=["wrt"], dkey="wrt")
    lo = pvs("lo")
    for d in range(2):
        l0 = lo[:, d * 32:d * 32 + 16]
        l1 = lo[:, d * 32 + 16:d * 32 + 32]
        tmpc = lbs[:, 3 * d + 2, :]
        Sc.op("dve", lambda e, o=tmpc, a=l0, b=l1: e.tensor_tensor(o, a, b, ALU.subtract), r=["pv"], w=[f"lbt{d}"])
        Sc.op("act", lambda e, o=lbs[:, 3 * d, :], i=tmpc: e.activation(o, i, AF.Sigmoid), r=[f"lbt{d}"], w=[f"lb{d}"])
        Sc.op("act", lambda e, o=lbs[:, 3 * d + 1, :], i=tmpc: e.activation(o, i, AF.Sigmoid, scale=-1.0),
              r=[f"lbt{d}"], w=[f"oml{d}"])
        Sc.op("dve", lambda e, o=tmpc, i=lbs[:, 3 * d + 1, :]: e.tensor_scalar(o, i, -1.0, None, ALU.mult),
              r=[f"oml{d}"], w=[f"lbt{d}"])
    LBK = ["lb0", "oml0", "lbt0", "lb1", "oml1", "lbt1"]

    XT = [A.f32(D) for _ in range(4)]
    XNT = A.f32(16, 512)
    XNB = A.bf16(16, 512)
    WS = [A.bf16(16, 256) for _ in range(2)]
    QS = A.bf16(16, 512)
    VT = A.bf16(4, D)
    TMP = [A.f32(512) for _ in range(8)]
    QT2 = [A.bf16(512) for _ in range(2)]
    KT = A.bf16(512)
    SCT2 = [A.bf16(4, 128) for _ in range(2)]
    KDT2 = [A.bf16(4, 128) for _ in range(2)]
    SF = A.f32(16, 128)
    SB2 = [A.bf16(128) for _ in range(2)]
    CIN = A.bf16(8, 512)
    HB = [A.f32(542) for _ in range(2)]
    OST = [A.f32(512) for _ in range(2)]
    STAT = A.f32(4, 32)
    ESM2 = [A.f32(2, 4) for _ in range(2)]
    RR = A.f32(512)
    MEANB = A.f32(512)
    assert A.off <= ARENA_WORDS, A.off
    ws_ctr = [0]

    ws_last = [None]

    wsc = nc.dram_tensor("wsc", [88, 128, 16 * 256], BF16, kind="Internal").ap()
    wsc_ids = {}

    def load_slab(src_ap, ncols=256, nk=16, sid=None):
        i = ws_ctr[0] % 2
        ws_ctr[0] += 1
        key = f"ws{i}"
        dst = WS[i][:, 0:nk, 0:ncols]
        first = sid not in wsc_ids
        if first:
            wsc_ids[sid] = len(wsc_ids)
        n = wsc_ids[sid]
        scr = wsc[n, :, 0:nk * ncols].rearrange("p (k c) -> p k c", k=nk)
        if first:
            Sc.dma("pool", dst, src_ap.rearrange("(kc p) c -> p kc c", p=128), w=[key], dkey=key, after=ws_last[0])
        else:
            Sc.dma("pool", dst, scr, r=[f"wsc{n}"], w=[key], dkey=key, after=ws_last[0])
        marker = ([], None, None)
        Sc.ops["pool"].append(marker)
        ws_last[0] = marker
        if first:
            Sc.dma("sp", scr, dst, r=[key], w=[f"wsc{n}"], dkey="wscst")
        return WS[i], key

    def mm(out_, lhsT, rhs, start, stop, r, w, inc):
        Sc.op("pe", lambda e: e.matmul(out_, lhsT, rhs, start=start, stop=stop), r=r, w=w, inc=inc)

    def tr(out_, in_, r, w, inc, kp=128):
        idn = ident[0:kp, 0:kp]
        Sc.op("pe", lambda e: e.transpose(out_, in_, idn), r=list(r) + ["cst"], w=w, inc=inc)

    def act(out_, in_, func, r, w, bias=None, scale=None):
        kw = {}
        if bias is not None:
            kw["bias"] = bias
        if scale is not None:
            kw["scale"] = scale
        Sc.op("act", lambda e: e.activation(out_, in_, func, **kw), r=r, w=w)

    def ts(eng, out_, in0, s1, s2, op0, op1, r, w):
        if op1 is None:
            Sc.op(eng, lambda e: e.tensor_scalar(out_, in0, s1, None, op0), r=r, w=w)
        else:
            Sc.op(eng, lambda e: e.tensor_scalar(out_, in0, s1, s2, op0, op1), r=r, w=w)

    def tt(eng, out_, in0, in1, op, r, w):
        Sc.op(eng, lambda e: e.tensor_tensor(out_, in0, in1, op), r=r, w=w)

    def stt(eng, out_, in0, scalar, in1, op0, op1, r, w):
        Sc.op(eng, lambda e: e.scalar_tensor_tensor(out_, in0, scalar, in1, op0, op1), r=r, w=w)

    psrot = [0]

    def next_ps(lo_=0, n=2):
        i = lo_ + (psrot[0] % n)
        psrot[0] += 1
        return PS[i], f"ps{i}"

    def ln_rows(tile, key, tagi):
        st = TMP[7][:, 0:24].rearrange("p (a b) -> p a b", a=4)
        mv = STAT[:, tagi, 0:2]
        rs = STAT[:, tagi, 2:3]
        for c4 in range(4):
            Sc.op("dve", lambda e, o=st[:, c4, :], i=tile[:, c4 * 512:(c4 + 1) * 512]: e.bn_stats(o, i),
                  r=[key], w=["tmp7"])
        Sc.op("dve", lambda e: e.bn_aggr(mv, TMP[7][:, 0:24]), r=["tmp7"], w=[f"mv{tagi}"])
        ts("dve", rs, mv[:, 1:2], EPS, None, ALU.add, None, r=[f"mv{tagi}"], w=[f"rs{tagi}"])
        act(rs, rs, AF.Sqrt, r=[f"rs{tagi}"], w=[f"rs{tagi}"])
        Sc.op("dve", lambda e: e.reciprocal(rs, rs), r=[f"rs{tagi}"], w=[f"rs{tagi}"])
        ts("dve", tile, tile, mv[:, 0:1], rs, ALU.subtract, ALU.mult, r=[key, f"mv{tagi}", f"rs{tagi}"], w=[key])

    def front(j, want_f32):
        for i in range(4):
            Sc.dma("sp", XT[i], x[j * 512 + i * 128:j * 512 + (i + 1) * 128, :], w=[f"XT{i}"], dkey=f"xt{i}")
            ln_rows(XT[i], f"XT{i}", i)
        for c in range(16):
            ps, pk = next_ps(2, 2)
            for i in range(4):
                tr(ps[:, i * 128:(i + 1) * 128], XT[i][:, c * 128:(c + 1) * 128], r=[f"XT{i}"], w=[pk], inc=(i == 3))
            if want_f32:
                act(XNT[:, c, :], ps, AF.Identity, r=[pk, "pv"], w=[f"xnt{c}"], bias=pvs("lnb", c), scale=pvs("lng", c))
                Sc.op("pool", lambda e, o=XNB[:, c, :], i_=XNT[:, c, :]: e.tensor_copy(o, i_), r=[f"xnt{c}"], w=[f"xnb{c}"])
            else:
                act(XNB[:, c, :], ps, AF.Identity, r=[pk, "pv"], w=[f"xnb{c}"], bias=pvs("lnb", c), scale=pvs("lng", c))

    XNBK = [f"xnb{c}" for c in range(16)]

    def proj_fm(col0, consume):
        wsl, wk = load_slab(w_in[:, col0:col0 + 256], sid=("in", col0))
        for mi in range(2):
            ps, pk = next_ps(0, 2)
            for k in range(16):
                mm(ps, wsl[:, k, mi * 128:(mi + 1) * 128], XNB[:, k, :], k == 0, k == 15,
                   r=[wk, XNBK[k]], w=[pk], inc=(k == 15))
            consume(mi, ps, pk)

    def proj_q():
        for sl in range(8):
            def cons(mi, ps, pk, sl=sl):
                h = sl * 2 + mi
                act(QS[:, h, :], ps, AF.Silu, r=[pk], w=[f"qs{h}"])
            proj_fm(0 * D + sl * 256, cons)

    def proj_v():
        for sl in range(8):
            wsl, wk = load_slab(w_in[:, 3 * D + sl * 256:3 * D + (sl + 1) * 256], sid=("in", 3 * D + sl * 256))
            for i in range(4):
                ps, pk = next_ps(0, 2)
                for k in range(16):
                    mm(ps[:, 0:256], XNB[:, k, i * 128:(i + 1) * 128], wsl[:, k, :], k == 0, k == 15,
                       r=[wk, XNBK[k]], w=[pk], inc=(k == 15))
                Sc.op("act", lambda e, o=VT[:, i, sl * 256:(sl + 1) * 256], p_=ps[:, 0:256]: e.copy(o, p_),
                      r=[pk], w=[f"vt{i}"])

    def scan_dir(j, d, emit_o):
        lb = lbs[:, 3 * d, :]
        oml = lbs[:, 3 * d + 1, :]
        noml = lbs[:, 3 * d + 2, :]
        fcol = (1 + d) * D
        chunks = [0, 1, 2, 3] if d == 0 else [3, 2, 1, 0]
        last = 127 if d == 0 else 0
        mask = maskF if d == 0 else maskB
        sg, g, kk, dd, ep, kd = TMP[0], TMP[1], TMP[2], TMP[4], TMP[5], TMP[1]
        bpre = TMP[3]
        if d == 0:
            bb, en, KBB, KEN = TMP[3], TMP[6], "tmp3", "tmp6"
        else:
            bb, en, KBB, KEN = TMP[6], TMP[3], "tmp6", "tmp3"
        bb3 = bb.rearrange("p (c t) -> p c t", c=4)
        bpre3 = bpre.rearrange("p (c t) -> p c t", c=4)
        dd3 = dd.rearrange("p (c t) -> p c t", c=4)
        ep3 = ep.rearrange("p (c t) -> p c t", c=4)
        kd3 = kd.rearrange("p (c t) -> p c t", c=4)
        OB = [(PS[6], "ps6"), (PS[3], "ps3")]
        PB = [(PS[7], "ps7"), (PS[2], "ps2")]
        for sl in range(8):
            wsl, wk = load_slab(w_in[:, fcol + sl * 256:fcol + (sl + 1) * 256], sid=("in", fcol + sl * 256))
            pss = []
            for mi in range(2):
                ps, pk = next_ps(0, 2)
                for k in range(16):
                    mm(ps, wsl[:, k, mi * 128:(mi + 1) * 128], XNB[:, k, :], k == 0, k == 15,
                       r=[wk, XNBK[k]], w=[pk], inc=(k == 15))
                pss.append((ps, pk))
            for mi in range(2):
                ps, pk = pss[mi]
                h = sl * 2 + mi
                QT, SCT, KDT, esm = QT2[mi], SCT2[mi], KDT2[mi], ESM2[mi]
                kq, ksc, kkd, kem, kel = f"qt{mi}", f"sct{mi}", f"kdt{mi}", f"emid{mi}", f"elast{mi}"
                act(sg, ps, AF.Sigmoid, r=[pk], w=["tmp0"])
                act(g, sg, AF.Ln, r=["tmp0"] + LBK, w=["tmp1"], bias=lb[:, h:h + 1], scale=oml[:, h:h + 1])
                ts("dve", kk, sg, noml[:, h:h + 1], oml[:, h:h + 1], ALU.mult, ALU.add, r=["tmp0"] + LBK, w=["tmp2"])
                Sc.op("dve", lambda e: e.tensor_tensor_scan(bpre, mreset, g, 0.0, ALU.mult, ALU.add),
                      r=["tmp1", "cst"], w=["tmp3"])
                if d == 1:
                    tt("dve", dd, g, bpre, ALU.subtract, r=["tmp1", "tmp3"], w=["tmp4"])
                    tt("dve", bb3, dd3, bpre3[:, :, 127:128].to_broadcast([128, 4, 128]), ALU.add,
                       r=["tmp4", "tmp3"], w=["tmp6"])
                act(esm[:, 0, :], bb3[:, :, 64], AF.Exp, r=[KBB], w=[kem])
                act(esm[:, 1, :], bb3[:, :, last], AF.Exp, r=[KBB], w=[kel])
                tt("dve", dd3, bb3, bb3[:, :, 64:65].to_broadcast([128, 4, 128]), ALU.subtract, r=[KBB], w=["tmp4"])
                act(ep, dd, AF.Exp, r=["tmp4"], w=["tmp5"])
                act(en, dd, AF.Exp, r=["tmp4"], w=[KEN], scale=-1.0)
                tt("pool", QT, QS[:, h, :], ep, ALU.mult, r=[f"qs{h}", "tmp5"], w=[kq])
                tt("dve", KT, kk, en, ALU.mult, r=["tmp2", KEN], w=["kt"])
                tt("dve", kd3, KT.rearrange("p (c t) -> p c t", c=4),
                   ep3[:, :, last:last + 1].to_broadcast([128, 4, 128]), ALU.mult, r=["kt", "tmp5"], w=["tmp1"])
                for c in range(4):
                    mm(PS[4][:, c * 128:(c + 1) * 128], KT[:, c * 128:(c + 1) * 128], QT[:, c * 128:(c + 1) * 128],
                       True, True, r=["kt", kq], w=["ps4"], inc=(c == 3))
                tt("dve", SCT, PS[4].rearrange("p (c t) -> p c t", c=4),
                   mask.rearrange("p (o t) -> p o t", o=1).to_broadcast([128, 4, 128]), ALU.mult,
                   r=["ps4", "cst"], w=[ksc])
                for c in range(4):
                    tr(PS[5][:, c * 128:(c + 1) * 128], kd[:, c * 128:(c + 1) * 128], r=["tmp1"], w=["ps5"], inc=(c == 3))
                Sc.op("act", lambda e, KDT=KDT: e.copy(KDT, PS[5].rearrange("p (c t) -> p c t", c=4)), r=["ps5"], w=[kkd])
            for c in chunks:
                for mi in range(2):
                    h = sl * 2 + mi
                    QT, SCT, KDT, esm, SB = QT2[mi], SCT2[mi], KDT2[mi], ESM2[mi], SB2[mi]
                    kq, ksc, kkd, kem, kel, ksb = f"qt{mi}", f"sct{mi}", f"kdt{mi}", f"emid{mi}", f"elast{mi}", f"sb{mi}"
                    ops, opk = OB[mi]
                    pps, ppk = PB[mi]
                    vk = VT[:, c, h * 128:(h + 1) * 128]
                    act(SB, SF[:, h, :], AF.Identity, r=[f"sf{h}", kem], w=[ksb], scale=esm[:, 0, c:c + 1])
                    mm(ops[:, c * 128:(c + 1) * 128], vk, SCT[:, c, :], True, False, r=[f"vt{c}", ksc], w=[opk], inc=False)
                    mm(ops[:, c * 128:(c + 1) * 128], SB, QT[:, c * 128:(c + 1) * 128], False, True,
                       r=[ksb, kq], w=[opk], inc=True)
                    mm(pps[:, 0:128], KDT[:, c, :], vk, True, True, r=[kkd, f"vt{c}"], w=[ppk], inc=True)
                    stt("dve", SF[:, h, :], SF[:, h, :], esm[:, 1, c:c + 1], pps[:, 0:128], ALU.mult, ALU.add,
                        r=[ppk, kel, f"sf{h}", ksb], w=[f"sf{h}"])
            for mi in range(2):
                emit_o(sl * 2 + mi, OB[mi][0], OB[mi][1])

    def reset_state():
        Sc.op("dve", lambda e: e.memset(SF, 0.0), r=[], w=[f"sf{h}" for h in range(16)])

    reset_state()
    for j in reversed(range(NB)):
        front(j, False)
        proj_q()
        proj_v()
        ostc = [0]

        def emit_o_A(h, ops, opk, j=j):
            i = ostc[0] % 2
            ostc[0] += 1
            Sc.op("act", lambda e: e.copy(OST[i], ops), r=[opk], w=[f"ost{i}"])
            Sc.dma("sp", obwd[h, :, j * 512:(j + 1) * 512], OST[i], r=[f"ost{i}"], w=[f"obwd{j}_{h}"], dkey=f"ost{i}")
        scan_dir(j, 1, emit_o_A)
        for sl in range(4):
            held = {}

            def cons_g(mi, ps, pk, sl=sl, held=held):
                t = TMP[mi]
                act(t, ps, AF.Sigmoid, r=[pk], w=[f"tmp{mi}"])
            proj_fm(5 * D + 1024 + sl * 256, cons_g)

            def cons_a(mi, ps, pk, sl=sl, j=j):
                i = ostc[0] % 2
                ostc[0] += 1
                ch = sl * 2 + mi
                tt("dve", OST[i], ps, TMP[mi], ALU.mult, r=[pk, f"tmp{mi}"], w=[f"ost{i}"])
                Sc.dma("sp", hsc[ch, :, j * 512:(j + 1) * 512], OST[i], r=[f"ost{i}"], w=[f"hsc{j}_{ch}"], dkey=f"ost{i}")
            proj_fm(5 * D + sl * 256, cons_a)
    Sc.barrier()
    ws_last[0] = None

    reset_state()
    CACC = TMP
    AIN = QS
    MIXB = VT.rearrange("p a b -> p (a b)").rearrange("p (a b) -> p a b", a=16)
    OBW = [XT[i].rearrange("p (a b) -> p a b", a=4) for i in range(4)]

    def fm_stats(srcs, keys, nfeat, sq, MEANB, RR, ksq, kmean, krr):
        n = len(srcs)
        for c in range(n):
            mm(PS[4], ones, srcs[c], c == 0, c == n - 1, r=[keys[c], "cst"], w=["ps4"], inc=(c == n - 1))
        for c in range(n):
            act(sq, srcs[c], AF.Square, r=[keys[c]], w=[ksq])
            mm(PS[5], ones, sq, c == 0, c == n - 1, r=[ksq, "cst"], w=["ps5"], inc=True)
        ts("dve", MEANB, PS[4], 1.0 / nfeat, None, ALU.mult, None, r=["ps4"], w=[kmean])
        tt("dve", RR, MEANB, MEANB, ALU.mult, r=[kmean], w=[krr])
        stt("dve", RR, PS[5], 1.0 / nfeat, RR, ALU.mult, ALU.subtract, r=["ps5", krr], w=[krr])
        ts("dve", RR, RR, EPS, None, ALU.add, None, r=[krr], w=[krr])
        act(RR, RR, AF.Sqrt, r=[krr], w=[krr])
        Sc.op("dve", lambda e: e.reciprocal(RR, RR), r=[krr], w=[krr])

    for j in range(NB):
        front(j, True)
        t0 = j * 512 - 15
        for ch in range(8):
            hb = HB[ch % 2]
            hk = f"hb{ch % 2}"
            lo_t = max(t0, 0)
            hi_t = min(t0 + 542, S)
            if lo_t > t0 or hi_t < t0 + 542:
                Sc.op("pool", lambda e, hb=hb: e.memset(hb, 0.0), r=[], w=[hk])
            Sc.dma("sp", hb[:, lo_t - t0:hi_t - t0], hsc[ch, :, lo_t:hi_t],
                   r=[f"hsc{jj}_{ch}" for jj in range(max(j - 1, 0), min(j + 2, NB))], w=[hk], dkey=hk)
            cw_o = PVO["cvw"][0] + ch * 31
            acc = CACC[ch]
            ts("dve", acc, hb[:, 0:512], pv[:, cw_o:cw_o + 1], pvs("cvb", ch), ALU.mult, ALU.add,
               r=[hk, "pv"], w=[f"tmp{ch}"])
            for k in range(1, 31):
                Sc.op("dve", lambda e, acc=acc, hb=hb, k=k, cw_o=cw_o: e.scalar_tensor_tensor(
                    acc, hb[:, k:k + 512], pv[:, cw_o + k:cw_o + k + 1], acc, ALU.mult, ALU.add),
                    r=[hk] if k < 30 else [hk], w=[f"tmp{ch}"])
        fm_stats([CACC[c] for c in range(8)], [f"tmp{c}" for c in range(8)], 1024.0, OST[0], MEANB, RR, "ost0", "meanb", "rr")
        for ch in range(8):
            tt("dve", CACC[ch], CACC[ch], MEANB, ALU.subtract, r=["meanb"], w=[f"tmp{ch}"])
            tt("dve", CACC[ch], CACC[ch], RR, ALU.mult, r=["rr"], w=[f"tmp{ch}"])
            act(CIN[:, ch, :], CACC[ch], AF.Silu, r=[f"tmp{ch}", "pv"], w=[f"cin{ch}"],
                bias=pvs("clb", ch), scale=pvs("clg", ch))
        proj_q()
        proj_v()
        for i in range(4):
            Sc.dma("sp", OBW[i], obwd[4 * i:4 * i + 4, :, j * 512:(j + 1) * 512].rearrange("h p s -> p h s"),
                   r=[f"obwd{j}_{h}" for h in range(4 * i, 4 * i + 4)], w=[f"XT{i}"], dkey=f"xt{i}")

        def emit_o_B(h, ops, opk):
            tt("dve", OBW[h // 4][:, h % 4, :], ops, OBW[h // 4][:, h % 4, :], ALU.add, r=[opk], w=[f"XT{h // 4}"])
        scan_dir(j, 0, emit_o_B)
        sq = OST[0]
        for h in range(16):
            act(sq, OBW[h // 4][:, h % 4, :], AF.Square, r=[f"XT{h // 4}"], w=["ost0"])
            mm(PS[5], ones, sq, h == 0, h == 15, r=["ost0", "cst"], w=["ps5"], inc=True)
        ts("dve", RR, PS[5], 1.0 / D, EPS, ALU.mult, ALU.add, r=["ps5"], w=["rr"])
        act(RR, RR, AF.Sqrt, r=["rr"], w=["rr"])
        Sc.op("dve", lambda e: e.reciprocal(RR, RR), r=["rr"], w=["rr"])
        for sl in range(8):
            def cons_og(mi, ps, pk, sl=sl):
                h = sl * 2 + mi
                t = TMP[mi]
                act(t, ps, AF.Silu, r=[pk], w=[f"tmp{mi}"])
                tt("dve", TMP[2 + mi], OBW[h // 4][:, h % 4, :], RR, ALU.mult, r=[f"XT{h // 4}", "rr"], w=[f"tmp{2 + mi}"])
                stt("dve", AIN[:, h, :], t, pvs("hgg", h), TMP[2 + mi], ALU.mult, ALU.mult,
                    r=[f"tmp{mi}", f"tmp{2 + mi}", "pv"], w=[f"qs{h}"])
            proj_fm(4 * D + sl * 256, cons_og)
        AINK = [f"qs{h}" for h in range(16)]
        CINK = [f"cin{c}" for c in range(8)]
        for sl in range(8):
            def cons_ga(mi, ps, pk):
                act(TMP[mi], ps, AF.Sigmoid, r=[pk], w=[f"tmp{mi}"])
            proj_fm(6 * D + sl * 256, cons_ga)

            def cons_gc(mi, ps, pk):
                act(TMP[2 + mi], ps, AF.Sigmoid, r=[pk], w=[f"tmp{2 + mi}"])
            proj_fm(7 * D + sl * 256, cons_gc)
            wsl, wk = load_slab(w_hg[:, sl * 256:(sl + 1) * 256], sid=("hg", sl))
            for mi in range(2):
                ps, pk = next_ps(0, 2)
                for k in range(16):
                    mm(ps, wsl[:, k, mi * 128:(mi + 1) * 128], AIN[:, k, :], k == 0, k == 15, r=[wk, AINK[k]], w=[pk], inc=(k == 15))
                tt("dve", TMP[mi], ps, TMP[mi], ALU.mult, r=[pk], w=[f"tmp{mi}"])
            wsl, wk = load_slab(w_co[:, sl * 256:(sl + 1) * 256], nk=8, sid=("co", sl))
            for mi in range(2):
                m = sl * 2 + mi
                ps, pk = next_ps(0, 2)
                for k in range(8):
                    mm(ps, wsl[:, k, mi * 128:(mi + 1) * 128], CIN[:, k, :], k == 0, k == 7, r=[wk, CINK[k]], w=[pk], inc=(k == 7))
                tt("dve", TMP[2 + mi], ps, TMP[2 + mi], ALU.mult, r=[pk], w=[f"tmp{2 + mi}"])
                tt("pool", MIXB[:, m, :], TMP[mi], TMP[2 + mi], ALU.add, r=[f"tmp{mi}", f"tmp{2 + mi}"], w=[f"vt{m // 4}"])
        for sl in range(8):
            wsl, wk = load_slab(w_out[:, sl * 256:(sl + 1) * 256], sid=("out", sl))
            for mi in range(2):
                m = sl * 2 + mi
                ps, pk = next_ps(0, 2)
                for k in range(16):
                    mm(ps, wsl[:, k, mi * 128:(mi + 1) * 128], MIXB[:, k, :], k == 0, k == 15,
                       r=[wk, f"vt{k // 4}"], w=[pk], inc=(k == 15))
                stt("dve", XNT[:, m, :], XNT[:, m, :], ALPHA, ps, ALU.mult, ALU.add, r=[pk], w=[f"xnt{m}"])
        fm_stats([XNT[:, c, :] for c in range(16)], [f"xnt{c}" for c in range(16)], float(D), OST[0], MEANB, RR, "ost0", "meanb", "rr")
        for m in range(16):
            tt("dve", XNT[:, m, :], XNT[:, m, :], MEANB, ALU.subtract, r=["meanb"], w=[f"xnt{m}"])
            tt("dve", XNT[:, m, :], XNT[:, m, :], RR, ALU.mult, r=["rr"], w=[f"xnt{m}"])
            act(XNT[:, m, :], XNT[:, m, :], AF.Identity, r=[f"xnt{m}", "pv"], w=[f"xnt{m}"],
                bias=pvs("l1b", m), scale=pvs("l1g", m))
        Sc.dma("sp", x1sc[:, :, j * 512:(j + 1) * 512].rearrange("c p s -> p c s"), XNT,
               r=[f"xnt{m}" for m in range(16)], w=[f"x1sc{j}"], dkey="x1st")
    Sc.barrier()

    A.off = CONST_END
    ACC = A.f32(16, 1024)
    X1B = A.bf16(16, 1024)
    actb_off = A.off
    ACTB = A.bf16(16, 1024)
    end_actb = A.off
    A.off = actb_off
    WPP = A.bf16(2, D)
    PT = A.bf16(2, 1024)
    assert A.off <= end_actb
    A.off = end_actb
    WB = [A.bf16(16, 256) for _ in range(3)]
    GBC = A.f32(1024)
    T = [A.f32(512) for _ in range(6)]
    ZT = A.f32(D)
    LG = A.f32(8, NE)
    GT = A.f32(1024)
    GTE = A.f32(1024)
    SM = A.f32(8, 16)
    assert A.off <= ARENA_WORDS, A.off
    ACTK = [f"actb{i}" for i in range(16)]
    wc = [0, 0]

    for gi in range(NG):
        tok0 = gi * 1024
        Sc.dma("sp", ACC, x1sc[:, :, tok0:tok0 + 1024].rearrange("c p s -> p c s"),
               r=[f"x1sc{2 * gi}", f"x1sc{2 * gi + 1}"], w=[f"acc{m}" for m in range(16)], dkey="accld")
        for m in range(16):
            Sc.op("pool", lambda e, m=m: e.tensor_copy(X1B[:, m, :], ACC[:, m, :]), r=[f"acc{m}"], w=[f"x1b{m}"])
        for blk in range(2):
            ps, pk = next_ps(6, 1)
            for k in range(16):
                mm(ps[0:NE, :], wrt[:, k, :], ACC[:, k, blk * 512:(blk + 1) * 512], k == 0, k == 15,
                   r=["wrt", f"acc{k}"], w=[pk], inc=(k == 15))
            ts("dve", GT[0:NE, blk * 512:(blk + 1) * 512], ps[0:NE, :], pv[0:NE, PVO["brt"][0]:PVO["brt"][0] + 1], None,
               ALU.add, None, r=[pk, "pv"], w=["gt"])
        for t8 in range(8):
            ps, pk = next_ps(7, 1)
            tr(ps[:, 0:NE], GT[0:NE, t8 * 128:(t8 + 1) * 128], r=["gt"], w=[pk], inc=True, kp=NE)
            lg = LG[:, t8, :]
            sm = SM[:, t8, :]
            lk = f"lg{t8}"
            sk = f"sm{t8}"
            Sc.op("dve", lambda e, lg=lg, ps=ps: e.tensor_copy(lg, ps[:, 0:NE]), r=[pk], w=[lk])
            Sc.op("dve", lambda e, lg=lg, sm=sm: e.max(sm[:, 0:8], lg), r=[lk], w=[sk])
            ts("dve", sm[:, 8:9], sm[:, 0:1], -1.0, None, ALU.mult, None, r=[sk], w=[sk])
            ts("dve", T[0][:, 0:NE], lg, sm[:, 3:4], None, ALU.is_ge, None, r=[lk, sk], w=["t0"])
            act(lg, lg, AF.Exp, r=[lk, sk], w=[lk], bias=sm[:, 8:9])
            tt("dve", lg, lg, T[0][:, 0:NE], ALU.mult, r=["t0", lk], w=[lk])
            Sc.op("dve", lambda e, lg=lg, sm=sm: e.reduce_sum(sm[:, 9:10], lg, mybir.AxisListType.X),
                  r=[lk], w=[sk])
            Sc.op("dve", lambda e, sm=sm: e.reciprocal(sm[:, 9:10], sm[:, 9:10]), r=[sk], w=[sk])
            ts("dve", lg, lg, sm[:, 9:10], None, ALU.mult, None, r=[sk, lk], w=[lk])
        for t8 in range(8):
            ps, pk = next_ps(7, 1)
            tr(ps[0:NE, 0:128], LG[:, t8, :], r=[f"lg{t8}"], w=[pk], inc=True)
            Sc.op("act", lambda e, t8=t8, ps=ps: e.copy(GT[0:NE, t8 * 128:(t8 + 1) * 128], ps[0:NE, 0:128]),
                  r=[pk], w=["gt"])
        Sc.dma("pool", WPP, w_pp.rearrange("(kc p) c -> p kc c", p=128), w=ACTK, dkey="wpp")
        for t8 in range(8):
            zt = ZT[:, 0:256]
            Sc.dma("sp", zt, pp[tok0 + t8 * 128:tok0 + (t8 + 1) * 128, :], w=["zt"], dkey="zt")
            for k in range(2):
                ps, pk = next_ps(7, 1)
                tr(ps[:, 0:128], zt[:, k * 128:(k + 1) * 128], r=["zt"], w=[pk], inc=True)
                Sc.op("act", lambda e, k=k, t8=t8, ps=ps: e.copy(PT[:, k, t8 * 128:(t8 + 1) * 128], ps[:, 0:128]),
                      r=[pk] + ACTK, w=["pt"])
        b1o = PVO["b1"][0]
        b2o = PVO["b2"][0]
        items = [("pg", sl) for sl in range(8)]
        for ex in range(NE):
            items += [("w1", ex, i) for i in range(16)] + [("w2", ex, s2) for s2 in range(8)]

        def load_item(it):
            wi = wc[0] % 3
            wc[0] += 1
            key = f"wb{wi}"
            rr_ = lambda ap: ap.rearrange("(kc p) c -> p kc c", p=128)
            if it[0] == "pg":
                Sc.dma("pool", WB[wi], rr_(w_pg[:, it[1] * 256:(it[1] + 1) * 256]), w=[key], dkey=key)
            elif it[0] == "w1":
                _, ex, i = it
                Sc.dma("pool", WB[wi][:, :, 0:128], rr_(w_e1[ex, :, i * 128:(i + 1) * 128]), w=[key], dkey=key)
                Sc.dma("pool", WB[wi][:, :, 128:256], rr_(w_e1[ex, :, D + i * 128:D + (i + 1) * 128]), w=[key], dkey=key)
            else:
                _, ex, s2 = it
                Sc.dma("pool", WB[wi], rr_(w_e2[ex, :, s2 * 256:(s2 + 1) * 256]), w=[key], dkey=key)
            return WB[wi], key

        def compute_item(it, wt, wk):
            if it[0] == "pg":
                sl = it[1]
                for mi in range(2):
                    m = sl * 2 + mi
                    for blk in range(2):
                        bs = slice(blk * 512, (blk + 1) * 512)
                        ps, pk = next_ps(0, 2)
                        for k in range(16):
                            mm(ps, wt[:, k, mi * 128:(mi + 1) * 128], X1B[:, k, bs], k == 0, k == 15,
                               r=[wk, f"x1b{k}"], w=[pk], inc=(k == 15))
                        act(T[0], ps, AF.Sigmoid, r=[pk], w=["t0"])
                        ps2, pk2 = next_ps(2, 2)
                        for k in range(2):
                            mm(ps2, WPP[:, k, m * 128:(m + 1) * 128], PT[:, k, bs], k == 0, k == 1,
                               r=ACTK + ["pt"], w=[pk2], inc=(k == 1))
                        tt("dve", T[0], T[0], ps2, ALU.mult, r=[pk2, "t0"], w=["t0"])
                        stt("dve", ACC[:, m, bs], ACC[:, m, bs], ALPHA, T[0], ALU.mult, ALU.add,
                            r=["t0", f"x1b{m}", f"acc{m}"], w=[f"acc{m}"])
            elif it[0] == "w1":
                _, ex, i = it
                if i == 0:
                    ts("dve", GTE[0:NE, :], GT[0:NE, :], ident[0:NE, ex:ex + 1], None, ALU.mult, None,
                       r=["gt", "cst"], w=["gte"])
                    for blk in range(2):
                        bs = slice(blk * 512, (blk + 1) * 512)
                        ps, pk = next_ps(6, 1)
                        mm(ps, ones[0:NE, :], GTE[0:NE, bs], True, True, r=["cst", "gte"], w=[pk], inc=True)
                        Sc.op("act", lambda e, ps=ps, bs=bs: e.copy(GBC[:, bs], ps), r=[pk], w=[f"gbc{blk}"])
                for blk in range(2):
                    bs = slice(blk * 512, (blk + 1) * 512)
                    psg, pkg = next_ps(0, 2)
                    for k in range(16):
                        mm(psg, wt[:, k, 0:128], X1B[:, k, bs], k == 0, k == 15, r=[wk, f"x1b{k}"], w=[pkg], inc=(k == 15))
                    psl, pkl = next_ps(2, 2)
                    for k in range(16):
                        mm(psl, wt[:, k, 128:256], X1B[:, k, bs], k == 0, k == 15, r=[wk, f"x1b{k}"], w=[pkl], inc=(k == 15))
                    bg = pv[:, b1o + ex * 32 + i:b1o + ex * 32 + i + 1]
                    bl = pv[:, b1o + ex * 32 + 16 + i:b1o + ex * 32 + 16 + i + 1]
                    ts("dve", T[1], psg, bg, 7.0, ALU.add, ALU.min, r=[pkg, "pv"], w=["t1"])
                    act(T[2], T[1], AF.Sigmoid, r=["t1"], w=["t2"], scale=1.702)
                    act(T[3], psl, AF.Identity, r=[pkl, "pv"], w=["t3"], bias=bl)
                    ts("dve", T[3], T[3], 7.0, -7.0, ALU.min, ALU.max, r=["t3"], w=["t3"])
                    tt("dve", T[1], T[1], T[2], ALU.mult, r=["t1", "t2"], w=["t1"])
                    stt("dve", ACTB[:, i, bs], T[3], 1.0, T[1], ALU.add, ALU.mult, r=["t1", "t3", "pt"], w=[f"actb{i}"])
            else:
                _, ex, s2 = it
                for mi in range(2):
                    m = s2 * 2 + mi
                    for blk in range(2):
                        bs = slice(blk * 512, (blk + 1) * 512)
                        ps, pk = next_ps(4, 2)
                        for k in range(16):
                            mm(ps, wt[:, k, mi * 128:(mi + 1) * 128], ACTB[:, k, bs], k == 0, k == 15,
                               r=[wk, f"actb{k}"], w=[pk], inc=(k == 15))
                        b2 = pv[:, b2o + ex * 16 + m:b2o + ex * 16 + m + 1]
                        stt("dve", T[4 + blk], ps, b2, GBC[:, bs], ALU.add, ALU.mult, r=[pk, "pv", f"gbc{blk}"], w=[f"t{4 + blk}"])
                        tt("dve", ACC[:, m, bs], ACC[:, m, bs], T[4 + blk], ALU.add, r=[f"t{4 + blk}", f"acc{m}"], w=[f"acc{m}"])

        loaded = [load_item(items[0]), load_item(items[1])]
        for n, it in enumerate(items):
            if n + 2 < len(items):
                loaded.append(load_item(items[n + 2]))
            wt, wk = loaded[n]
            compute_item(it, wt, wk)
        for blk in range(2):
            bs = slice(blk * 512, (blk + 1) * 512)
            fm_stats([ACC[:, m, bs] for m in range(16)], [f"acc{m}" for m in range(16)], float(D),
                     T[0], T[1], T[2], "t0", "t1", "t2")
            for m in range(16):
                tt("dve", ACC[:, m, bs], ACC[:, m, bs], T[1], ALU.subtract, r=["t1", f"acc{m}"], w=[f"acc{m}"])
                tt("pool", ACC[:, m, bs], ACC[:, m, bs], T[2], ALU.mult, r=["t2", f"acc{m}"], w=[f"acc{m}"])
                act(ACC[:, m, bs], ACC[:, m, bs], AF.Identity, r=[f"acc{m}", "pv"], w=[f"acc{m}"],
                    bias=pvs("l2b", m), scale=pvs("l2g", m))
            for i in range(4):
                c0 = blk * 512 + i * 128
                for q4 in range(4):
                    ps, pk = next_ps(0, 4)
                    for mm_ in range(4):
                        m = q4 * 4 + mm_
                        tr(ps[:, mm_ * 128:(mm_ + 1) * 128], ACC[:, m, c0:c0 + 128], r=[f"acc{m}"], w=[pk], inc=(mm_ == 3))
                    Sc.op("act", lambda e, ps=ps, q4=q4: e.copy(ZT[:, q4 * 512:(q4 + 1) * 512], ps), r=[pk], w=["zt"])
                Sc.dma("sp", out[tok0 + c0:tok0 + c0 + 128, :], ZT, r=["zt"], w=["outst"], dkey="outst")
    Sc.barrier()
    Sc.emit()
    return nc


def _v16(v):
    return np.ascontiguousarray(np.asarray(v, np.float32).reshape(-1, 128).T)


def _prep_shared(inp, NE):
    PVO, PVN = pv_layout(NE)
    pv = np.zeros((128, PVN), np.float32)

    def put(name, a, c0=0):
        o, n = PVO[name]
        pv[:, o + c0:o + c0 + a.shape[1]] = a

    put("lng", _v16(inp["ln_emb_g"]))
    put("lnb", _v16(inp["ln_emb_b"]))
    lbd = np.asarray(inp["lower_bounds"], np.float32)
    for d in range(2):
        for sl in range(2):
            put("lo", _v16(lbd[d, sl]), d * 32 + sl * 16)
    put("hgg", _v16(inp["hg_norm_g"][0]))
    put("cvb", _v16(inp["conv_b"][0]))
    put("clg", _v16(inp["conv_ln_g"][0]))
    put("clb", _v16(inp["conv_ln_b"][0]))
    put("l1g", _v16(inp["ln1_g"][0]))
    put("l1b", _v16(inp["ln1_b"][0]))
    put("l2g", _v16(inp["ln2_g"][0]))
    put("l2b", _v16(inp["ln2_b"][0]))
    cw = np.asarray(inp["conv_w"][0], np.float32)
    put("cvw", np.ascontiguousarray(cw.T.reshape(8, 128, 31).transpose(1, 0, 2)).reshape(128, 8 * 31))
    b1 = np.asarray(inp["b_exp1"][0], np.float32)[:NE]
    put("b1", np.ascontiguousarray(b1.reshape(NE, 32, 128).transpose(2, 0, 1)).reshape(128, NE * 32))
    b2 = np.asarray(inp["b_exp2"][0], np.float32)[:NE]
    put("b2", np.ascontiguousarray(b2.reshape(NE, 16, 128).transpose(2, 0, 1)).reshape(128, NE * 16))
    o = PVO["brt"][0]
    pv[0:NE, o] = np.asarray(inp["b_router"][0], np.float32)[:NE]
    cst = np.zeros((128, 1024), np.float32)
    cst[:, 0:128] = np.eye(128, dtype=np.float32)
    cst[:, 128:256] = 1.0
    ii = np.arange(128)
    cst[:, 256:384] = (ii[:, None] <= ii[None, :]).astype(np.float32)
    cst[:, 384:512] = (ii[:, None] >= ii[None, :]).astype(np.float32)
    mr = np.ones(512, np.float32)
    mr[::128] = 0.0
    cst[:, 512:1024] = mr[None, :]
    sh = {
        "w_in": np.ascontiguousarray(inp["w_in"][0], np.float32),
        "w_hg": np.ascontiguousarray(inp["w_hg_out"][0], np.float32),
        "w_co": np.ascontiguousarray(inp["w_conv_out"][0], np.float32),
        "w_out": np.ascontiguousarray(inp["w_out"][0], np.float32),
        "w_rt": np.ascontiguousarray(inp["w_router"][0][:, :NE], np.float32),
        "w_e1": np.ascontiguousarray(inp["w_exp1"][0][:NE], np.float32),
        "w_e2": np.ascontiguousarray(inp["w_exp2"][0][:NE], np.float32),
        "w_pg": np.ascontiguousarray(inp["w_ple_gate"][0], np.float32),
        "w_pp": np.ascontiguousarray(inp["w_ple_proj"][0], np.float32),
        "pv": pv,
        "cst": cst,
    }
    return sh


_NC_CACHE = {}


def kernel(**inputs):
    S, NE = 4096, 32
    inp = {k: np.asarray(v) for k, v in inputs.items()}
    sh = _prep_shared(inp, NE)
    seqs = [(inp["x_prompt"][b], inp["p_prompt"][0, b]) for b in range(2)]
    seqs += [(inp["x_sample"][b], inp["p_sample"][0, b]) for b in range(4)]
    seqs += [seqs[0], seqs[1]]
    if (S, NE) not in _NC_CACHE:
        _NC_CACHE[(S, NE)] = build(S, NE)
    nc = _NC_CACHE[(S, NE)]
    in_maps = []
    for c in range(8):
        m = dict(sh)
        m["x"] = np.ascontiguousarray(seqs[c][0], np.float32)
        m["p"] = np.ascontiguousarray(seqs[c][1], np.float32)
        in_maps.append(m)
    res = run_bass_kernel_spmd(nc, in_maps, core_ids=list(range(8)))
    outs = [np.asarray(r["out"], np.float32) for r in res.results]
    y_prompt = np.stack(outs[0:2], axis=0)
    y_sample = np.stack(outs[2:6], axis=0)
    return (y_prompt, y_sample)
```
